# Optimizing a Trainium2 kernel written in Bass

```python
import math
import jax, jax.numpy as jnp
from jax import lax
import numpy as np

D_MODEL = 1024
BATCH = 16
SEQ = 2048
DEPTH = 1
DEC_BATCH = 128
DEC_SEQ = 8
PAST_LEN = 8192
PAGE_SIZE = 128

CONV_CH = 512
CONV_GROUPS = 8
CONV_W = 3
N_HEADS = 8
QK_NOPE = 64
QK_ROPE = 32
V_DIM = 64
Q_LORA = 256
KV_LORA = 128
ATTN_W = N_HEADS * V_DIM
MIX_W = CONV_CH + ATTN_W
IN_COLS = 3 * CONV_CH + Q_LORA + KV_LORA + QK_ROPE
ROPE_BASE = 10000.0
SOFTMAX_SCALE = (QK_NOPE + QK_ROPE) ** -0.5
Q_BLOCK = 128
NEG_INF = -1e30
N_EXPERTS = 256
TOP_K = 8
N_GROUPS = 8
TOPK_GROUPS = 4
D_EXPERT = 256
D_SHARED = 256
ROUTED_SCALE = 2.5
EXPERT_BLOCK = 128
NORM_EPS = 1e-6
LN_EPS = 1e-5
DEEPNORM_ALPHA = (2.0 * DEPTH) ** 0.25
DEEPNORM_BETA = (8.0 * DEPTH) ** -0.25

kernel_name = 'hymba_conv_mla_moe_deepnorm_step'


def rms_norm(x, g):
    xf = x.astype(jnp.float32)
    y = xf * lax.rsqrt(jnp.mean(xf * xf, axis=-1, keepdims=True) + NORM_EPS)
    return (y * g.astype(jnp.float32)).astype(x.dtype)


def layer_norm(x, g, b):
    xf = x.astype(jnp.float32)
    mu = jnp.mean(xf, axis=-1, keepdims=True)
    var = jnp.mean(jnp.square(xf - mu), axis=-1, keepdims=True)
    y = (xf - mu) * lax.rsqrt(var + LN_EPS) * g.astype(jnp.float32) + b.astype(jnp.float32)
    return y.astype(x.dtype)


def rope_tables(pos, dtype):
    half = QK_ROPE // 2
    inv = ROPE_BASE ** (-jnp.arange(half, dtype=jnp.float32) / half)
    ang = pos.astype(jnp.float32)[:, None] * inv[None, :]
    return jnp.cos(ang).astype(dtype), jnp.sin(ang).astype(dtype)


def apply_rope(x, cos, sin):
    x1, x2 = jnp.split(x, 2, axis=-1)
    c, s = cos[:, None, :], sin[:, None, :]
    return jnp.concatenate([x1 * c - x2 * s, x1 * s + x2 * c], axis=-1)


def mixer_inputs(x, lw, pos):
    b, t, _ = x.shape
    h = jnp.einsum('btd,dc->btc', x, lw['w_in'])
    o1, o2, o3 = CONV_CH, 2 * CONV_CH, 3 * CONV_CH
    o4 = o3 + Q_LORA
    o5 = o4 + KV_LORA
    b_gate, c_gate, x_conv = h[..., :o1], h[..., o1:o2], h[..., o2:o3]
    q_a, c_kv, k_pe = h[..., o3:o4], h[..., o4:o5], h[..., o5:]
    u = c_gate * x_conv
    q = jnp.einsum('btr,rf->btf', rms_norm(q_a, lw['q_norm']), lw['w_uq'])
    q = q.reshape(b, t, N_HEADS, QK_NOPE + QK_ROPE)
    cos, sin = rope_tables(pos, x.dtype)
    q_pe = apply_rope(q[..., QK_NOPE:], cos, sin)
    k_pe = apply_rope(k_pe[:, :, None, :], cos, sin)[:, :, 0, :]
    c_kv = rms_norm(c_kv, lw['kv_norm'])
    q_lat = jnp.einsum('bthn,chn->bthc', q[..., :QK_NOPE], lw['w_uk'])
    return b_gate, u, q_lat, q_pe, c_kv, k_pe


def short_conv(u_ext, conv_w, t):
    y = conv_w[0] * u_ext[:, 0:t]
    for j in range(1, CONV_W):
        y = y + conv_w[j] * u_ext[:, j:j + t]
    return y


def latent_scores(q_lat, q_pe, c_kv, k_pe):
    s = jnp.einsum('bqhc,bkc->bhqk', q_lat, c_kv, preferred_element_type=jnp.float32)
    s = s + jnp.einsum('bqhr,bkr->bhqk', q_pe, k_pe, preferred_element_type=jnp.float32)
    return s * SOFTMAX_SCALE


def prompt_attention(q_lat, q_pe, c_kv, k_pe):
    b, s = q_lat.shape[:2]
    nb = s // Q_BLOCK
    kpos = jnp.arange(s)

    def block(i):
        q0 = i * Q_BLOCK
        ql = lax.dynamic_slice_in_dim(q_lat, q0, Q_BLOCK, axis=1)
        qp = lax.dynamic_slice_in_dim(q_pe, q0, Q_BLOCK, axis=1)
        sc = latent_scores(ql, qp, c_kv, k_pe)
        qpos = q0 + jnp.arange(Q_BLOCK)
        sc = jnp.where(kpos[None, :] <= qpos[:, None], sc, NEG_INF)
        p = jax.nn.softmax(sc, axis=-1).astype(c_kv.dtype)
        return jnp.einsum('bhqk,bkc->bqhc', p, c_kv)

    out = lax.map(block, jnp.arange(nb))
    return out.transpose(1, 0, 2, 3, 4).reshape(b, s, N_HEADS, KV_LORA)


def sample_attention(q_lat, q_pe, c_past, kpe_past, c_new, kpe_new):
    t = q_lat.shape[1]
    past_len = c_past.shape[1]
    s_past = latent_scores(q_lat, q_pe, c_past, kpe_past)
    causal = jnp.arange(t)[None, :] <= jnp.arange(t)[:, None]
    s_new = jnp.where(causal, latent_scores(q_lat, q_pe, c_new, kpe_new), NEG_INF)
    p = jax.nn.softmax(jnp.concatenate([s_past, s_new], axis=-1), axis=-1).astype(c_new.dtype)
    return (jnp.einsum('bhqk,bkc->bqhc', p[..., :past_len], c_past)
            + jnp.einsum('bhqk,bkc->bqhc', p[..., past_len:], c_new))


def route(x2, w_router, router_bias):
    n = x2.shape[0]
    s = jax.nn.sigmoid(jnp.einsum('nd,de->ne', x2, w_router, preferred_element_type=jnp.float32))
    sb = s + router_bias.astype(jnp.float32)
    gscore = lax.top_k(sb.reshape(n, N_GROUPS, N_EXPERTS // N_GROUPS), 2)[0].sum(-1)
    _, gidx = lax.top_k(gscore, TOPK_GROUPS)
    gmask = jax.nn.one_hot(gidx, N_GROUPS, dtype=jnp.float32).sum(1) > 0
    emask = jnp.repeat(gmask, N_EXPERTS // N_GROUPS, axis=1)
    _, idx = lax.top_k(jnp.where(emask, sb, -jnp.inf), TOP_K)
    w = jnp.take_along_axis(s, idx, axis=1)
    w = w / jnp.sum(w, axis=-1, keepdims=True) * ROUTED_SCALE
    return idx.astype(jnp.int32), w


def moe_routed(x2, idx, wts, w_gate, w_up, w_down):
    n, d = x2.shape
    a = n * TOP_K
    n_blocks = -(-a // EXPERT_BLOCK) + N_EXPERTS
    flat_e = idx.reshape(a)
    order = jnp.argsort(flat_e)
    e_sorted = flat_e[order]
    counts = jnp.zeros((N_EXPERTS,), jnp.int32).at[flat_e].add(1)
    padded = (counts + EXPERT_BLOCK - 1) // EXPERT_BLOCK * EXPERT_BLOCK
    pad_end = jnp.cumsum(padded)
    pad_start = pad_end - padded
    start = jnp.cumsum(counts) - counts
    dest = pad_start[e_sorted] + jnp.arange(a, dtype=jnp.int32) - start[e_sorted]
    slot_tok = jnp.full((n_blocks * EXPERT_BLOCK,), n, jnp.int32).at[dest].set((order // TOP_K).astype(jnp.int32))
    slot_w = jnp.zeros((n_blocks * EXPERT_BLOCK,), x2.dtype).at[dest].set(wts.reshape(a)[order].astype(x2.dtype))
    block_pos = jnp.arange(n_blocks, dtype=jnp.int32) * EXPERT_BLOCK
    block_e = jnp.minimum(jnp.searchsorted(pad_end, block_pos, side='right'), N_EXPERTS - 1).astype(jnp.int32)
    x_pad = jnp.concatenate([x2, jnp.zeros((1, d), x2.dtype)], axis=0)

    def body(acc, blk):
        tok, w, e = blk
        xb = x_pad[tok]
        h = jax.nn.silu(xb @ w_gate[e]) * (xb @ w_up[e])
        return acc.at[tok].add((h @ w_down[e]) * w[:, None]), None

    acc, _ = lax.scan(body, jnp.zeros((n + 1, d), x2.dtype),
                      (slot_tok.reshape(n_blocks, EXPERT_BLOCK),
                       slot_w.reshape(n_blocks, EXPERT_BLOCK), block_e))
    return acc[:n]


def channel_mixer(x, lw):
    b, t, d = x.shape
    x2 = x.reshape(b * t, d)
    idx, wts = route(x2, lw['w_router'], lw['router_bias'])
    routed = moe_routed(x2, idx, wts, lw['w_gate'], lw['w_up'], lw['w_down'])
    shared = (jax.nn.silu(x2 @ lw['ws_gate']) * (x2 @ lw['ws_up'])) @ lw['ws_down']
    return (routed + shared).reshape(b, t, d)


def finish_layer(x, b_gate, conv_y, attn_lat, lw):
    b, t, _ = x.shape
    y_conv = rms_norm(b_gate * conv_y, lw['g_conv'])
    o = jnp.einsum('bthc,chv->bthv', attn_lat, lw['w_uv']).reshape(b, t, ATTN_W)
    y_attn = rms_norm(o, lw['g_attn'])
    mix = jnp.einsum('btm,md->btd', jnp.concatenate([y_conv, y_attn], axis=-1), lw['w_o'])
    x = layer_norm(DEEPNORM_ALPHA * x + mix, lw['ln1_g'], lw['ln1_b'])
    return layer_norm(DEEPNORM_ALPHA * x + channel_mixer(x, lw), lw['ln2_g'], lw['ln2_b'])


def prompt_layer(x, lw):
    b, s, _ = x.shape
    b_gate, u, q_lat, q_pe, c_kv, k_pe = mixer_inputs(x, lw, jnp.arange(s, dtype=jnp.int32))
    u_ext = jnp.concatenate([jnp.zeros((b, CONV_W - 1, CONV_CH), u.dtype), u], axis=1)
    conv_y = short_conv(u_ext, lw['conv_w'], s)
    attn_lat = prompt_attention(q_lat, q_pe, c_kv, k_pe)
    x = finish_layer(x, b_gate, conv_y, attn_lat, lw)
    return x, c_kv, k_pe, u_ext[:, s:]


def sample_layer(x, c_past, kpe_past, conv_state, lw):
    b, t, _ = x.shape
    past_len = c_past.shape[1]
    pos = past_len + jnp.arange(t, dtype=jnp.int32)
    b_gate, u, q_lat, q_pe, c_kv, k_pe = mixer_inputs(x, lw, pos)
    u_ext = jnp.concatenate([conv_state.astype(u.dtype), u], axis=1)
    conv_y = short_conv(u_ext, lw['conv_w'], t)
    attn_lat = sample_attention(q_lat, q_pe, c_past.astype(c_kv.dtype), kpe_past.astype(k_pe.dtype), c_kv, k_pe)
    x = finish_layer(x, b_gate, conv_y, attn_lat, lw)
    return x, c_kv, k_pe, u_ext[:, t:]


def setup_inputs(seed: int = 0) -> dict:
    key = jax.random.key(seed)
    ks = jax.random.split(key, 32)
    n_pages = PAST_LEN // PAGE_SIZE
    n_used = DEC_BATCH * n_pages
    n_pool = n_used + n_used // 4

    def dense(k, shape, fan_in, scale=1.0):
        return jax.random.normal(k, shape, jnp.float32) * (scale * fan_in ** -0.5)

    def gain(k, shape):
        return 1.0 + 0.02 * jax.random.normal(k, shape, jnp.float32)

    def small(k, shape, s=0.02):
        return s * jax.random.normal(k, shape, jnp.float32)

    perm = jax.random.permutation(ks[5], n_pool).astype(jnp.int32)
    page_table = perm[:n_used].reshape(DEC_BATCH, n_pages)
    L, E = DEPTH, N_EXPERTS
    return {
        'x_prompt': jax.random.normal(ks[0], (BATCH, SEQ, D_MODEL), jnp.float32),
        'x_sample': jax.random.normal(ks[1], (DEC_BATCH, DEC_SEQ, D_MODEL), jnp.float32),
        'cache_kv_latent': jax.random.normal(ks[2], (L, n_pool, PAGE_SIZE, KV_LORA), jnp.float32),
        'cache_k_rope': jax.random.normal(ks[3], (L, n_pool, PAGE_SIZE, QK_ROPE), jnp.float32),
        'state_conv': jax.random.normal(ks[4], (L, DEC_BATCH, CONV_W - 1, CONV_CH), jnp.float32),
        'page_table': page_table,
        'w_in': dense(ks[6], (L, D_MODEL, IN_COLS), D_MODEL),
        'conv_w': dense(ks[7], (L, CONV_W, CONV_CH), CONV_W),
        'q_norm': gain(ks[8], (L, Q_LORA)),
        'w_uq': dense(ks[9], (L, Q_LORA, N_HEADS * (QK_NOPE + QK_ROPE)), Q_LORA),
        'kv_norm': gain(ks[10], (L, KV_LORA)),
        'w_uk': dense(ks[11], (L, KV_LORA, N_HEADS, QK_NOPE), KV_LORA),
        'w_uv': dense(ks[12], (L, KV_LORA, N_HEADS, V_DIM), KV_LORA, DEEPNORM_BETA),
        'g_conv': gain(ks[13], (L, CONV_CH)),
        'g_attn': gain(ks[14], (L, ATTN_W)),
        'w_o': dense(ks[15], (L, MIX_W, D_MODEL), MIX_W, DEEPNORM_BETA),
        'ln1_g': gain(ks[16], (L, D_MODEL)),
        'ln1_b': small(ks[17], (L, D_MODEL)),
        'w_router': dense(ks[18], (L, D_MODEL, E), D_MODEL),
        'router_bias': small(ks[19], (L, E), 0.01),
        'w_gate': dense(ks[20], (L, E, D_MODEL, D_EXPERT), D_MODEL, DEEPNORM_BETA),
        'w_up': dense(ks[21], (L, E, D_MODEL, D_EXPERT), D_MODEL, DEEPNORM_BETA),
        'w_down': dense(ks[22], (L, E, D_EXPERT, D_MODEL), D_EXPERT, DEEPNORM_BETA),
        'ws_gate': dense(ks[23], (L, D_MODEL, D_SHARED), D_MODEL, DEEPNORM_BETA),
        'ws_up': dense(ks[24], (L, D_MODEL, D_SHARED), D_MODEL, DEEPNORM_BETA),
        'ws_down': dense(ks[25], (L, D_SHARED, D_MODEL), D_SHARED, DEEPNORM_BETA),
        'ln2_g': gain(ks[26], (L, D_MODEL)),
        'ln2_b': small(ks[27], (L, D_MODEL)),
    }


def reference(x_prompt, x_sample, cache_kv_latent, cache_k_rope, state_conv, page_table,
              w_in, conv_w, q_norm, w_uq, kv_norm, w_uk, w_uv, g_conv, g_attn, w_o,
              ln1_g, ln1_b, w_router, router_bias, w_gate, w_up, w_down,
              ws_gate, ws_up, ws_down, ln2_g, ln2_b):
    db = x_sample.shape[0]
    xp, xs = x_prompt, x_sample
    kv_p, kpe_p, cs_p, kv_s, kpe_s, cs_s = [], [], [], [], [], []
    for l in range(DEPTH):
        lw = {'w_in': w_in[l], 'conv_w': conv_w[l], 'q_norm': q_norm[l], 'w_uq': w_uq[l],
              'kv_norm': kv_norm[l], 'w_uk': w_uk[l], 'w_uv': w_uv[l], 'g_conv': g_conv[l],
              'g_attn': g_attn[l], 'w_o': w_o[l], 'ln1_g': ln1_g[l], 'ln1_b': ln1_b[l],
              'w_router': w_router[l], 'router_bias': router_bias[l], 'w_gate': w_gate[l],
              'w_up': w_up[l], 'w_down': w_down[l], 'ws_gate': ws_gate[l], 'ws_up': ws_up[l],
              'ws_down': ws_down[l], 'ln2_g': ln2_g[l], 'ln2_b': ln2_b[l]}
        xp, c_new, k_new, conv_new = prompt_layer(xp, lw)
        kv_p.append(c_new); kpe_p.append(k_new); cs_p.append(conv_new)
        c_past = cache_kv_latent[l][page_table].reshape(db, -1, KV_LORA)
        kpe_past = cache_k_rope[l][page_table].reshape(db, -1, QK_ROPE)
        xs, c_new, k_new, conv_new = sample_layer(xs, c_past, kpe_past, state_conv[l], lw)
        kv_s.append(c_new); kpe_s.append(k_new); cs_s.append(conv_new)
    return (xp, xs, jnp.stack(kv_p), jnp.stack(kpe_p), jnp.stack(cs_p),
            jnp.stack(kv_s), jnp.stack(kpe_s), jnp.stack(cs_s))
```

```python
import numpy as np
import ml_dtypes
import concourse.bass as bass
import concourse.mybir as mybir
from concourse.bass_utils import run_bass_kernel_spmd

F32 = mybir.dt.float32
BF16 = mybir.dt.bfloat16
I32 = mybir.dt.int32
AF = mybir.ActivationFunctionType
ALU = mybir.AluOpType
AX = mybir.AxisListType

NCORES = 8
D = 1024
SEQ = 2048
NSEQ = 2
NQB = SEQ // 128
NPT = NSEQ * NQB
NT = NPT + 1
NTOK = NT * 128
DB = 16
DT = 8
PAST = 8192
NPAGE = 64
NPOOL = 10240
CONV_CH = 512
QL = 256
KVL = 128
ROPE = 32
NH = 8
NOPE = 64
VD = 64
INC = 1952
NE = 256
CAP = 256
NSLOT = NE * CAP
ALPHA = 2.0 ** 0.25
SCALE = (NOPE + ROPE) ** -0.5
NORM_EPS = 1e-6
LN_EPS = 1e-5
ROUTED_SCALE = 2.5

import os
STAGE = int(os.environ.get('KSTAGE', '5'))
SUB = int(os.environ.get('KSUB', '9'))
KNT = int(os.environ.get('KNT', '0'))
KDBG = int(os.environ.get('KDBG', '99'))
NPOOLD = NPOOL


class Eng:
    def __init__(self, nc, name, e):
        self.e = e
        self.name = name
        self.sem = nc.alloc_semaphore(name + "_prog")
        self.cnt = 0
        self.waited = {}


class Buf:
    def __init__(self, t):
        self.t = t
        self.w = None
        self.r = []

    def __getitem__(self, k):
        return self.t[k]


class KB:
    def __init__(self, nc):
        self.nc = nc
        self.pe = Eng(nc, "pe", nc.tensor)
        self.act = Eng(nc, "act", nc.scalar)
        self.dve = Eng(nc, "dve", nc.vector)
        self.pool = Eng(nc, "pool", nc.gpsimd)
        self.sp = Eng(nc, "sp", nc.sync)
        self.dma_sems = [[nc.alloc_semaphore(f"dma{i}"), 0] for i in range(40)]
        self.dma_rr = 0
        self.nbuf = 0

    def sb(self, shape, dt, name=None):
        self.nbuf += 1
        return Buf(self.nc.alloc_sbuf_tensor("s_" + (name or f"sb{self.nbuf}"), list(shape), dt))

    def ps(self, name=None):
        self.nbuf += 1
        return Buf(self.nc.alloc_psum_tensor("p_" + (name or f"ps{self.nbuf}"), [128, 512], F32))

    def alias(self, parent, view):
        b = Buf(view)
        b.r = list(parent.r)
        if parent.w is not None:
            b.r.append(parent.w)
        for ch in getattr(parent, "children", []):
            b.r.extend(ch.r)
            if ch.w is not None:
                b.r.append(ch.w)
        if not hasattr(parent, "children"):
            parent.children = []
        parent.children.append(b)
        return b

    def need(self, X, deps):
        for d in deps:
            if d is None:
                continue
            sem, val = d
            if sem is X.sem and X.name == "pe":
                continue
            key = id(sem)
            if X.waited.get(key, 0) < val:
                X.e.wait_ge(sem, val)
                X.waited[key] = val

    def _deps(self, reads, writes, extra):
        deps = list(extra)
        for b in reads:
            if b.w is not None:
                deps.append(b.w)
        for b in writes:
            if b.w is not None:
                deps.append(b.w)
            deps.extend(b.r)
        return deps

    def _commit(self, tok, reads, writes):
        for b in reads:
            b.r.append(tok)
            if len(b.r) > 24:
                b.r = b.r[-24:]
        for b in writes:
            b.w = tok
            b.r = []

    def op(self, X, fn, reads=(), writes=(), extra=()):
        self.need(X, self._deps(reads, writes, extra))
        inst = fn()
        X.cnt += 1
        inst.then_inc(X.sem, 1)
        tok = (X.sem, X.cnt)
        self._commit(tok, reads, writes)
        return tok

    def group(self, X, fns, reads=(), writes=(), extra=()):
        self.need(X, self._deps(reads, writes, extra))
        inst = None
        for fn in fns:
            inst = fn()
        X.cnt += 1
        inst.then_inc(X.sem, 1)
        tok = (X.sem, X.cnt)
        self._commit(tok, reads, writes)
        return tok

    def dma(self, Q, fn, reads=(), writes=(), extra=()):
        slot = self.dma_sems[self.dma_rr % len(self.dma_sems)]
        self.dma_rr += 1
        deps = self._deps(reads, writes, extra)
        if slot[1] > 0:
            deps.append((slot[0], slot[1]))
        self.need(Q, deps)
        inst = fn()
        slot[1] += 16
        inst.then_inc(slot[0], 16)
        tok = (slot[0], slot[1])
        self._commit(tok, reads, writes)
        return tok

    def finish(self):
        deps = [(s, v) for s, v in self.dma_sems if v > 0]
        for E in (self.pe, self.act, self.dve, self.pool):
            if E.cnt > 0:
                deps.append((E.sem, E.cnt))
        self.need(self.sp, deps)


def build_program():
    nc = bass.Bass("TRN2", target_bir_lowering=False)
    kb = KB(nc)
    pe, act, dve, pool, sp = kb.pe, kb.act, kb.dve, kb.pool, kb.sp

    in_names = []

    def din(name, shape, dt=F32):
        in_names.append(name)
        return nc.dram_tensor(name, list(shape), dt, kind="ExternalInput").ap()

    def dout(name, shape, dt=F32):
        return nc.dram_tensor(name, list(shape), dt, kind="ExternalOutput").ap()

    xp = din("xp", [NPT * 128, D])
    xs = din("xs", [128, D])
    w_in_d = din("w_in", [D, INC])
    w_uq_d = din("w_uq", [QL, 768])
    w_ukT_d = din("w_ukT", [128, 8, 128])
    w_uv_d = din("w_uv", [128, 8, 128])
    rowmask_d = din("rowmask", [128, 4])
    w_o_d = din("w_o", [D, D])
    gmix_d = din("gmix", [128, 8])
    qnorm_d = din("qnorm", [128, 2])
    kvnorm_d = din("kvnorm", [1, 128])
    convw_d = din("convw", [128, 4, 3])
    ln1g_d = din("ln1g", [1, D])
    ln1b_d = din("ln1b", [1, D])
    ln2g_d = din("ln2g", [1, D])
    ln2b_d = din("ln2b", [1, D])
    w_router_d = din("w_router", [D, NE])
    rbias_d = din("rbias", [1, NE])
    ws_gate_d = din("ws_gate", [D, 256])
    ws_up_d = din("ws_up", [D, 256])
    ws_down_d = din("ws_down", [256, D])
    if STAGE >= 5:
        NED = int(os.environ.get('KNE', str(NE)))
        w_gate_d = din("w_gate", [NED, 128, 2048])
        w_up_d = din("w_up", [NED, 128, 2048])
        w_down_d = din("w_down", [NED, 128, 2048])
    if STAGE >= 3:
        cache_d = din("cache_all", [NPOOLD * 128, KVL + ROPE])
        smask_d = din("smask", [128, DB, DT])
        pcol_d = din("pcol", [128, 1])
    ptab_d = din("ptab", [1, DB * NPAGE], I32)
    sconv_d = din("sconv", [DB, 2, CONV_CH])
    cosp_d = din("cosp", [128, NQB, 16])
    sinp_d = din("sinp", [128, NQB, 16])
    coss_d = din("coss", [128, 16])
    sins_d = din("sins", [128, 16])
    identf_d = din("identf", [128, 128])
    cmask_d = din("cmask", [128, 128])

    ustrict_d = din("ustrict", [128, 128])
    ebase_d = din("ebase", [1, NE])
    dummycol_d = din("dummycol", [128, 1])
    tokid_d = din("tokid", [128, NT, 16], I32)
    x1tab_d = nc.dram_tensor("x1tab", [NTOK + 128, D], BF16).ap()
    pre2_d = nc.dram_tensor("pre2", [NTOK, D], F32).ap()
    slot_tok_d = nc.dram_tensor("slot_tok", [NSLOT + 128, 16], I32).ap()
    yslots_d = nc.dram_tensor("yslots", [NSLOT + 128, D], BF16).ap()
    x1tab_b = Buf(x1tab_d); pre2_b = Buf(pre2_d); slot_tok_b = Buf(slot_tok_d); yslots_b = Buf(yslots_d)

    y_p = dout("y_p", [NPT * 128, D])
    y_s = dout("y_s", [128, D])
    kv_p = dout("kv_p", [NPT * 128, KVL])
    kr_p = dout("kr_p", [NPT * 128, ROPE])
    cs_p = dout("cs_p", [NSEQ, 2, CONV_CH])
    kv_s = dout("kv_s", [128, KVL])
    kr_s = dout("kr_s", [128, ROPE])
    cs_s = dout("cs_s", [DB, 2, CONV_CH])

    identf = kb.sb([128, 128], F32, "identf")
    identb = kb.sb([128, 128], BF16, "identb")
    ones_f = kb.sb([128, 128], F32, "ones_f")
    ones_b = kb.sb([128, 128], BF16, "ones_b")
    cmask = kb.sb([128, 128], BF16, "cmask")
    w_in = kb.sb([128, 8, INC], BF16, "w_in")
    w_uq = kb.sb([128, 2, 768], BF16, "w_uq")
    w_ukT = kb.sb([128, 8, 128], BF16, "w_ukT")
    w_uv = kb.sb([128, 8, 128], BF16, "w_uv")
    rowmask = kb.sb([128, 4], F32, "rowmask")
    qTm = kb.sb([128, 8, 128], BF16, "qTm")
    qnorm = kb.sb([128, 2], F32, "qnorm")
    kvnorm = kb.sb([128, 128], F32, "kvnorm")
    convw = kb.sb([128, 4, 3], F32, "convw")
    cosp = kb.sb([128, NQB, 16], F32, "cosp")
    sinp = kb.sb([128, NQB, 16], F32, "sinp")
    coss = kb.sb([128, 16], F32, "coss")
    sins = kb.sb([128, 16], F32, "sins")

    kb.dma(sp, lambda: sp.e.dma_start(out=identf[:], in_=identf_d), writes=[identf])
    kb.dma(pool, lambda: pool.e.dma_start(out=identb[:], in_=identf_d), writes=[identb])
    kb.dma(pool, lambda: pool.e.dma_start(out=cmask[:], in_=cmask_d), writes=[cmask])
    kb.op(dve, lambda: dve.e.memset(ones_f[:], 1.0), writes=[ones_f])
    kb.op(dve, lambda: dve.e.memset(ones_b[:], 1.0), writes=[ones_b])
    for k in range(8):
        kb.dma(pool, lambda k=k: pool.e.dma_start(out=w_in[:, k, :], in_=w_in_d[k * 128:(k + 1) * 128, :]),
               writes=[w_in])
    kb.dma(sp, lambda: sp.e.dma_start(out=qnorm[:], in_=qnorm_d), writes=[qnorm])
    kb.dma(pool, lambda: pool.e.dma_start(out=w_ukT[:], in_=w_ukT_d), writes=[w_ukT])
    kb.dma(pool, lambda: pool.e.dma_start(out=w_uv[:], in_=w_uv_d), writes=[w_uv])
    kb.dma(sp, lambda: sp.e.dma_start(out=rowmask[:], in_=rowmask_d), writes=[rowmask])
    kb.dma(sp, lambda: sp.e.dma_start(out=kvnorm[:], in_=kvnorm_d.partition_broadcast(128)), writes=[kvnorm])
    kb.dma(sp, lambda: sp.e.dma_start(out=convw[:], in_=convw_d), writes=[convw])
    kb.dma(sp, lambda: sp.e.dma_start(out=cosp[:], in_=cosp_d), writes=[cosp])
    kb.dma(sp, lambda: sp.e.dma_start(out=sinp[:], in_=sinp_d), writes=[sinp])
    kb.dma(sp, lambda: sp.e.dma_start(out=coss[:], in_=coss_d), writes=[coss])
    kb.dma(sp, lambda: sp.e.dma_start(out=sins[:], in_=sins_d), writes=[sins])

    PS = [kb.ps(f"psb{i}") for i in range(8)]
    xin = [kb.sb([128, D], F32, f"xin{i}") for i in range(2)]
    xT = kb.sb([128, 8, 128], BF16, "xT")
    ctmp = kb.sb([128, 128], F32, "ctmp")
    uT = kb.sb([128, 4, 130], F32, "uT")
    uS = kb.sb([128, 4, DB, 10], F32, "uS")
    ytmp = kb.sb([128, 128], F32, "ytmp")
    zf = kb.sb([128, 128], F32, "zf")
    zsq = kb.sb([128, 128], F32, "zsq")
    mixT = kb.sb([128, 8, 128], BF16, "mixT")
    stat = kb.sb([128, 8], F32, "stat")
    junk = kb.sb([128, 512], F32, "junk")
    ckvn = kb.sb([128, 128], F32, "ckvn")
    ckvb = kb.sb([128, 128], BF16, "ckvb")
    kr = kb.sb([128, 32], F32, "kr")
    rt = kb.sb([128, 4, 16], F32, "rt")
    kr4 = kb.sb([128, 128], BF16, "kr4")
    ckvT = [kb.sb([128, SEQ], BF16, f"ckvT{s}") for s in range(NSEQ)]
    ckvE = [kb.sb([128, NQB, 128], BF16, f"ckvE{s}") for s in range(NSEQ)]
    kpeT = [kb.sb([128, SEQ], BF16, f"kpeT{s}") for s in range(NSEQ)]
    ckvT_s = kb.sb([128, 128], BF16, "ckvT_s")
    ckvE_s = kb.sb([128, 128], BF16, "ckvE_s")
    kpeT_s = kb.sb([128, 128], BF16, "kpeT_s")

    qa_bf = kb.sb([128, 256], BF16, "qa_bf")
    qaT = kb.sb([128, 2, 128], BF16, "qaT")
    q_sb = kb.sb([128, 768], F32, "q_sb")
    q_bf = kb.sb([128, 768], BF16, "q_bf")
    qrt = kb.sb([128, 4, 8, 16], F32, "qrt")
    qT = kb.sb([128, 6, 128], BF16, "qT")
    q_latT = kb.sb([128, 8, 128], BF16, "q_latT")
    pT = [kb.sb([128, 1024], BF16, f"pT{i}") for i in range(2)]
    rden = kb.sb([128, 1024], F32, "rden")
    o_latT = kb.sb([128, 8, 128], BF16, "o_latT")
    osq = kb.sb([128, 512], F32, "osq")
    stat2 = kb.sb([128, 16], F32, "stat2")
    pre = kb.sb([128, D], F32, "pre")
    x1 = kb.sb([128, D], F32, "x1")
    w_o = kb.sb([128, 8, D], BF16, "w_o")
    w_o_f = pre
    junk2 = kb.sb([128, D], F32, "junk2")
    for k in range(2):
        kb.dma(sp, lambda k=k: sp.e.dma_start(out=junk2[:, 0:768], in_=w_uq_d[k * 128:(k + 1) * 128, :]),
               writes=[junk2])
        kb.op(dve, lambda k=k: dve.e.tensor_scalar(out=w_uq[:, k, :], in0=junk2[:, 0:768],
                                                   scalar1=qnorm[:, k:k + 1], scalar2=None, op0=ALU.mult),
              reads=[junk2, qnorm], writes=[w_uq])
    gmix = kb.sb([128, 8], F32, "gmix")
    ln1g = kb.sb([128, D], F32, "ln1g")
    ln1b = kb.sb([128, D], F32, "ln1b")
    kb.dma(sp, lambda: sp.e.dma_start(out=gmix[:], in_=gmix_d), writes=[gmix])
    kb.dma(sp, lambda: sp.e.dma_start(out=ln1g[:], in_=ln1g_d.partition_broadcast(128)), writes=[ln1g])
    kb.dma(sp, lambda: sp.e.dma_start(out=ln1b[:], in_=ln1b_d.partition_broadcast(128)), writes=[ln1b])
    for k in range(8):
        kb.dma(sp, lambda k=k: sp.e.dma_start(out=w_o_f[:], in_=w_o_d[k * 128:(k + 1) * 128, :]), writes=[w_o_f])
        kb.op(dve, lambda k=k: dve.e.tensor_scalar(out=w_o[:, k, :], in0=w_o_f[:], scalar1=gmix[:, k:k + 1],
                                                   scalar2=None, op0=ALU.mult),
              reads=[w_o_f, gmix], writes=[w_o])

    def psb(buf, lo, hi):
        return buf.t[:].bitcast(BF16)[:, lo:hi]

    def phase_a(t):
        sample = (t == NPT)
        s, qb = (t // NQB, t % NQB) if not sample else (0, 0)
        xi = xin[t % 2]
        src = xs if sample else xp[t * 128:(t + 1) * 128, :]
        kb.dma(sp, lambda: sp.e.dma_start(out=xi[:], in_=src), writes=[xi])
        for half in range(2):
            pb = PS[half]
            kb.group(pe, [lambda k=k, pb=pb, half=half: pe.e.transpose(
                out=pb[:, (k - 4 * half) * 128:(k - 4 * half + 1) * 128],
                in_=xi[:, k * 128:(k + 1) * 128], identity=identf[:]) for k in range(4 * half, 4 * half + 4)],
                reads=[xi, identf], writes=[pb])
            kb.op(act, lambda pb=pb, half=half: act.e.activation(
                out=xT[:, 4 * half:4 * half + 4, :], in_=pb[:, :].rearrange("p (k n) -> p k n", k=4),
                func=AF.Copy), reads=[pb], writes=[xT])
        pq = PS[2]
        kb.group(pe, [lambda k=k: pe.e.matmul(pq[:, 0:416], lhsT=xT[:, k, :], rhs=w_in[:, k, 1536:1952],
                                             start=(k == 0), stop=(k == 7)) for k in range(8)],
                 reads=[xT, w_in], writes=[pq])
        cw = convw
        for j in range(4):
            pc = PS[3 + (j % 2)]
            for g in range(3):
                col0 = g * 512 + j * 128
                kb.group(pe, [lambda k=k, g=g, col0=col0, pc=pc: pe.e.matmul(
                    pc[:, g * 128:(g + 1) * 128], lhsT=w_in[:, k, col0:col0 + 128], rhs=xT[:, k, :],
                    start=(k == 0), stop=(k == 7)) for k in range(8)],
                    reads=[xT, w_in], writes=[pc])
            kb.op(act, lambda pc=pc: act.e.activation(out=ctmp[:], in_=pc[:, 128:256], func=AF.Copy),
                  reads=[pc], writes=[ctmp])
            if not sample:
                if qb == 0:
                    kb.op(dve, lambda j=j: dve.e.memset(uT[:, j, 0:2], 0.0), writes=[uT])
                kb.op(dve, lambda j=j, pc=pc: dve.e.tensor_tensor(out=uT[:, j, 2:130], in0=ctmp[:],
                                                                 in1=pc[:, 256:384], op=ALU.mult),
                      reads=[ctmp, pc], writes=[uT])
                kb.op(dve, lambda j=j: dve.e.tensor_scalar(out=ytmp[:], in0=uT[:, j, 0:128],
                                                           scalar1=cw[:, j, 0:1], scalar2=None, op0=ALU.mult),
                      reads=[uT, cw], writes=[ytmp])
                for i in (1, 2):
                    kb.op(dve, lambda j=j, i=i: dve.e.scalar_tensor_tensor(
                        out=ytmp[:], in0=uT[:, j, i:i + 128], scalar=cw[:, j, i:i + 1], in1=ytmp[:],
                        op0=ALU.mult, op1=ALU.add), reads=[uT, cw, ytmp], writes=[ytmp])
                if qb == NQB - 1:
                    with nc.allow_non_contiguous_dma(reason="tiny conv-state store"):
                        kb.dma(sp, lambda j=j: sp.e.dma_start(
                            out=cs_p[s, :, j * 128:(j + 1) * 128].rearrange("i c -> c i"),
                            in_=uT[:, j, 128:130]), reads=[uT])
                else:
                    kb.op(dve, lambda j=j: dve.e.tensor_copy(out=uT[:, j, 0:2], in_=uT[:, j, 128:130]),
                          reads=[uT], writes=[uT])
            else:
                with nc.allow_non_contiguous_dma(reason="tiny conv-state load"):
                    for i in range(2):
                        kb.dma(sp, lambda j=j, i=i: sp.e.dma_start(
                            out=uS[:, j, :, i:i + 1],
                            in_=sconv_d[:, i, j * 128:(j + 1) * 128].rearrange("b (c o) -> c b o", o=1)),
                            writes=[uS])
                kb.op(dve, lambda j=j, pc=pc: dve.e.tensor_tensor(
                    out=uS[:, j, :, 2:10], in0=ctmp[:].rearrange("p (b t) -> p b t", t=DT),
                    in1=pc[:, 256:384].rearrange("p (b t) -> p b t", t=DT), op=ALU.mult),
                    reads=[ctmp, pc], writes=[uS])
                yv = ytmp[:].rearrange("p (b t) -> p b t", t=DT)
                kb.op(dve, lambda j=j: dve.e.tensor_scalar(out=yv, in0=uS[:, j, :, 0:8],
                                                           scalar1=cw[:, j, 0:1], scalar2=None, op0=ALU.mult),
                      reads=[uS, cw], writes=[ytmp])
                for i in (1, 2):
                    kb.op(dve, lambda j=j, i=i: dve.e.scalar_tensor_tensor(
                        out=yv, in0=uS[:, j, :, i:i + 8], scalar=cw[:, j, i:i + 1], in1=yv,
                        op0=ALU.mult, op1=ALU.add), reads=[uS, cw, ytmp], writes=[ytmp])
                with nc.allow_non_contiguous_dma(reason="tiny conv-state store"):
                    for i in range(2):
                        kb.dma(sp, lambda j=j, i=i: sp.e.dma_start(
                            out=cs_s[:, i, j * 128:(j + 1) * 128].rearrange("b (c o) -> c b o", o=1),
                            in_=uS[:, j, :, 8 + i:9 + i]), reads=[uS])
            kb.op(dve, lambda pc=pc: dve.e.tensor_tensor(out=zf[:], in0=ytmp[:], in1=pc[:, 0:128], op=ALU.mult),
                  reads=[ytmp, pc], writes=[zf])
            kb.op(act, lambda j=j: act.e.activation(out=mixT[:, j, :], in_=zf[:], func=AF.Copy),
                  reads=[zf], writes=[mixT])
            kb.op(act, lambda: act.e.activation(out=zsq[:], in_=zf[:], func=AF.Square),
                  reads=[zf], writes=[zsq])
            kb.group(pe, [lambda j=j: pe.e.matmul(PS[5][:, 0:2], lhsT=zsq[:], rhs=ones_f[:, 0:2],
                                                 start=(j == 0), stop=(j == 3))],
                     reads=[zsq, ones_f], writes=[PS[5]])
        kb.op(dve, lambda: dve.e.tensor_copy(out=stat2[:, 0:1], in_=PS[5][:, 0:1]), reads=[PS[5]], writes=[stat2])
        kb.op(act, lambda: act.e.activation(out=junk[:, 0:256], in_=pq[:, 0:256], func=AF.Square,
                                            accum_out=stat[:, 0:1]), reads=[pq], writes=[junk, stat])
        kb.op(act, lambda: act.e.activation(out=junk[:, 256:384], in_=pq[:, 256:384], func=AF.Square,
                                            accum_out=stat[:, 1:2]), reads=[pq], writes=[junk, stat])
        kb.op(dve, lambda: dve.e.tensor_scalar(out=stat[:, 2:3], in0=stat[:, 0:1], scalar1=1.0 / QL,
                                               scalar2=NORM_EPS, op0=ALU.mult, op1=ALU.add),
              reads=[stat], writes=[stat])
        kb.op(dve, lambda: dve.e.tensor_scalar(out=stat[:, 3:4], in0=stat[:, 1:2], scalar1=1.0 / KVL,
                                               scalar2=NORM_EPS, op0=ALU.mult, op1=ALU.add),
              reads=[stat], writes=[stat])
        kb.op(act, lambda: act.e.activation(out=stat[:, 4:6], in_=stat[:, 2:4], func=AF.Sqrt),
              reads=[stat], writes=[stat])
        kb.op(dve, lambda: dve.e.reciprocal(out=stat[:, 6:8], in_=stat[:, 4:6]), reads=[stat], writes=[stat])
        kb.op(dve, lambda: dve.e.scalar_tensor_tensor(out=ckvn[:], in0=pq[:, 256:384], scalar=stat[:, 7:8],
                                                      in1=kvnorm[:], op0=ALU.mult, op1=ALU.mult),
              reads=[pq, stat, kvnorm], writes=[ckvn])
        kvo = kv_s if sample else kv_p[t * 128:(t + 1) * 128, :]
        kb.dma(sp, lambda: sp.e.dma_start(out=kvo, in_=ckvn[:]), reads=[ckvn])
        cE, cEv = (ckvE_s, ckvE_s[:, :]) if sample else (ckvE[s], ckvE[s][:, qb, :])
        kb.op(act, lambda: act.e.activation(out=cEv, in_=ckvn[:], func=AF.Copy), reads=[ckvn], writes=[cE])
        pt = PS[6]
        kb.group(pe, [lambda: pe.e.transpose(out=psb(pt, 0, 128), in_=cEv, identity=identb[:])],
                 reads=[cE, identb], writes=[pt])
        cT, cTv = (ckvT_s, ckvT_s[:, :]) if sample else (ckvT[s], ckvT[s][:, qb * 128:(qb + 1) * 128])
        kb.op(act, lambda: act.e.activation(out=cTv, in_=psb(pt, 0, 128), func=AF.Copy), reads=[pt], writes=[cT])
        cosv = coss[:, :] if sample else cosp[:, qb, :]
        sinv = sins[:, :] if sample else sinp[:, qb, :]
        tabs = [coss, sins] if sample else [cosp, sinp]
        x1 = pq[:, 384:400]
        x2 = pq[:, 400:416]
        kb.op(dve, lambda: dve.e.tensor_tensor(out=rt[:, 0, :], in0=x1, in1=cosv, op=ALU.mult),
              reads=[pq] + tabs, writes=[rt])
        kb.op(dve, lambda: dve.e.tensor_tensor(out=rt[:, 1, :], in0=x2, in1=sinv, op=ALU.mult),
              reads=[pq] + tabs, writes=[rt])
        kb.op(dve, lambda: dve.e.tensor_tensor(out=rt[:, 2, :], in0=x1, in1=sinv, op=ALU.mult),
              reads=[pq] + tabs, writes=[rt])
        kb.op(dve, lambda: dve.e.tensor_tensor(out=rt[:, 3, :], in0=x2, in1=cosv, op=ALU.mult),
              reads=[pq] + tabs, writes=[rt])
        kb.op(dve, lambda: dve.e.tensor_tensor(out=kr[:, 0:16], in0=rt[:, 0, :], in1=rt[:, 1, :], op=ALU.subtract),
              reads=[rt], writes=[kr])
        kb.op(dve, lambda: dve.e.tensor_tensor(out=kr[:, 16:32], in0=rt[:, 2, :], in1=rt[:, 3, :], op=ALU.add),
              reads=[rt], writes=[kr])
        kro = kr_s if sample else kr_p[t * 128:(t + 1) * 128, :]
        kb.dma(sp, lambda: sp.e.dma_start(out=kro, in_=kr[:]), reads=[kr])
        for rep in range(4):
            kb.op(act, lambda rep=rep: act.e.activation(out=kr4[:, rep * 32:(rep + 1) * 32], in_=kr[:],
                                                        func=AF.Copy), reads=[kr], writes=[kr4])
        kb.group(pe, [lambda: pe.e.transpose(out=psb(pt, 128, 256), in_=kr4[:], identity=identb[:])],
                 reads=[kr4, identb], writes=[pt])
        kT, kTv = (kpeT_s, kpeT_s[:, :]) if sample else (kpeT[s], kpeT[s][:, qb * 128:(qb + 1) * 128])
        kb.op(act, lambda: act.e.activation(out=kTv, in_=psb(pt, 128, 256), func=AF.Copy), reads=[pt], writes=[kT])
        if STAGE < 2:
            return
        if KDBG <= 0:
            return
        kb.op(act, lambda: act.e.activation(out=qa_bf[:], in_=pq[:, 0:256], func=AF.Copy), reads=[pq], writes=[qa_bf])
        p7 = PS[7]
        kb.group(pe, [lambda k=k: pe.e.transpose(out=psb(p7, k * 128, (k + 1) * 128),
                                                in_=qa_bf[:, k * 128:(k + 1) * 128], identity=identb[:])
                      for k in range(2)], reads=[qa_bf, identb], writes=[p7])
        kb.op(act, lambda: act.e.activation(out=qaT[:], in_=psb(p7, 0, 256).rearrange("p (k n) -> p k n", k=2),
                                            func=AF.Copy), reads=[p7], writes=[qaT])
        if KDBG <= 1:
            return
        kb.group(pe, [lambda k=k: pe.e.matmul(PS[0][:, 0:512], lhsT=qaT[:, k, :], rhs=w_uq[:, k, 0:512],
                                             start=(k == 0), stop=(k == 1)) for k in range(2)],
                 reads=[qaT, w_uq], writes=[PS[0]])
        kb.group(pe, [lambda k=k: pe.e.matmul(PS[1][:, 0:256], lhsT=qaT[:, k, :], rhs=w_uq[:, k, 512:768],
                                             start=(k == 0), stop=(k == 1)) for k in range(2)],
                 reads=[qaT, w_uq], writes=[PS[1]])
        kb.op(act, lambda: act.e.activation(out=q_bf[:, 0:512], in_=PS[0][:, 0:512], func=AF.Copy,
                                            scale=stat[:, 6:7]), reads=[PS[0], stat], writes=[q_bf])
        kb.op(act, lambda: act.e.activation(out=q_sb[:, 512:768], in_=PS[1][:, 0:256], func=AF.Copy,
                                            scale=stat[:, 6:7]), reads=[PS[1], stat], writes=[q_sb])
        if KDBG <= 2:
            return
        qv = q_sb[:, 512:768].rearrange("p (h r) -> p h r", h=NH)
        qo = q_bf[:, 512:768].rearrange("p (h r) -> p h r", h=NH)
        cb = cosv.unsqueeze(1).to_broadcast([128, NH, 16])
        sbv = sinv.unsqueeze(1).to_broadcast([128, NH, 16])
        qx1 = qv[:, :, 0:16]
        qx2 = qv[:, :, 16:32]
        kb.op(dve, lambda: dve.e.tensor_tensor(out=qrt[:, 0], in0=qx1, in1=cb, op=ALU.mult),
              reads=[q_sb] + tabs, writes=[qrt])
        kb.op(dve, lambda: dve.e.tensor_tensor(out=qrt[:, 1], in0=qx2, in1=sbv, op=ALU.mult),
              reads=[q_sb] + tabs, writes=[qrt])
        kb.op(dve, lambda: dve.e.tensor_tensor(out=qrt[:, 2], in0=qx1, in1=sbv, op=ALU.mult),
              reads=[q_sb] + tabs, writes=[qrt])
        kb.op(dve, lambda: dve.e.tensor_tensor(out=qrt[:, 3], in0=qx2, in1=cb, op=ALU.mult),
              reads=[q_sb] + tabs, writes=[qrt])
        kb.op(dve, lambda: dve.e.tensor_tensor(out=qo[:, :, 0:16], in0=qrt[:, 0], in1=qrt[:, 1], op=ALU.subtract),
              reads=[qrt], writes=[q_bf])
        kb.op(dve, lambda: dve.e.tensor_tensor(out=qo[:, :, 16:32], in0=qrt[:, 2], in1=qrt[:, 3], op=ALU.add),
              reads=[qrt], writes=[q_bf])
        if KDBG <= 3:
            return
        p3 = PS[3]
        kb.group(pe, [lambda k=k: pe.e.transpose(out=psb(p3, k * 128, (k + 1) * 128),
                                                in_=q_bf[:, k * 128:(k + 1) * 128], identity=identb[:])
                      for k in range(6)], reads=[q_bf, identb], writes=[p3])
        kb.op(act, lambda: act.e.activation(out=qT[:], in_=psb(p3, 0, 768).rearrange("p (k n) -> p k n", k=6),
                                            func=AF.Copy), reads=[p3], writes=[qT])
        if KDBG <= 4:
            return
        for half in range(2):
            pb = PS[half]
            for hh in range(4):
                h = 4 * half + hh
                kb.group(pe, [lambda h=h, hh=hh, pb=pb: pe.e.matmul(
                    pb[:, hh * 128:(hh + 1) * 128], lhsT=w_ukT[:, h, :],
                    rhs=qT[:, h // 2, :], start=True, stop=True)],
                    reads=[w_ukT, qT], writes=[pb])
            kb.op(act, lambda half=half, pb=pb: act.e.activation(
                out=q_latT[:, 4 * half:4 * half + 4, :], in_=pb[:, :].rearrange("p (k n) -> p k n", k=4),
                func=AF.Copy), reads=[pb], writes=[q_latT])
        for h in range(NH):
            kb.op(dve, lambda h=h: dve.e.tensor_scalar(out=qTm[:, h, :], in0=qT[:, 4 + h // 4, :],
                                                       scalar1=rowmask[:, h % 4:h % 4 + 1], scalar2=None,
                                                       op0=ALU.mult), reads=[qT, rowmask], writes=[qTm])

    def phase_b(t):
        s, qb = t // NQB, t % NQB
        for kb_i in range(qb + 1):
            bsel = kb_i % 2
            ks = slice(kb_i * 128, (kb_i + 1) * 128)
            for half in range(2):
                bank = PS[2 * bsel + half]
                fns = [lambda half=half, bank=bank, ks=ks: pe.e.matmul(
                    bank[:, 0:512], lhsT=ckvT[s][:, ks],
                    rhs=q_latT[:, 4 * half:4 * half + 4, :].rearrange("p k n -> p (k n)"),
                    start=True, stop=False)]
                for hh in range(4):
                    h = 4 * half + hh
                    fns.append(lambda hh=hh, h=h, bank=bank, ks=ks: pe.e.matmul(
                        bank[:, hh * 128:(hh + 1) * 128], lhsT=kpeT[s][:, ks],
                        rhs=qTm[:, h, :], start=False, stop=(hh == 3)))
                kb.group(pe, fns, reads=[ckvT[s], kpeT[s], q_latT, qTm], writes=[bank])
                kb.op(act, lambda half=half, bank=bank, bsel=bsel: act.e.activation(
                    out=pT[bsel][:, half * 512:(half + 1) * 512], in_=bank[:, 0:512], func=AF.Exp, scale=SCALE),
                    reads=[bank], writes=[pT[bsel]])
            if kb_i == qb:
                kb.op(dve, lambda bsel=bsel: dve.e.tensor_tensor(
                    out=pT[bsel][:, :].rearrange("p (h q) -> p h q", h=NH),
                    in0=pT[bsel][:, :].rearrange("p (h q) -> p h q", h=NH),
                    in1=cmask[:, :].unsqueeze(1).to_broadcast([128, NH, 128]), op=ALU.mult),
                    reads=[pT[bsel], cmask], writes=[pT[bsel]])
            for half in range(2):
                kb.group(pe, [lambda half=half, bsel=bsel, kb_i=kb_i: pe.e.matmul(
                    PS[4 + half][:, 0:512], lhsT=ckvE[s][:, kb_i, :], rhs=pT[bsel][:, half * 512:(half + 1) * 512],
                    start=(kb_i == 0), stop=(kb_i == qb))],
                    reads=[ckvE[s], pT[bsel]], writes=[PS[4 + half]])
                kb.group(pe, [lambda half=half, bsel=bsel, kb_i=kb_i: pe.e.matmul(
                    PS[6 + half][:, 0:512], lhsT=ones_b[:, :], rhs=pT[bsel][:, half * 512:(half + 1) * 512],
                    start=(kb_i == 0), stop=(kb_i == qb))],
                    reads=[ones_b, pT[bsel]], writes=[PS[6 + half]])
        for half in range(2):
            kb.op(dve, lambda half=half: dve.e.reciprocal(out=rden[:, half * 512:(half + 1) * 512],
                                                          in_=PS[6 + half][:, 0:512]),
                  reads=[PS[6 + half]], writes=[rden])
            kb.op(dve, lambda half=half: dve.e.tensor_tensor(
                out=o_latT[:, 4 * half:4 * half + 4, :].rearrange("p k n -> p (k n)"),
                in0=PS[4 + half][:, 0:512], in1=rden[:, half * 512:(half + 1) * 512], op=ALU.mult),
                reads=[PS[4 + half], rden], writes=[o_latT])


    def phase_bs():
        G = 8
        ptab_i = kb.alias(rden, rden.t[:].bitcast(I32))
        idx_f = pre
        idx_i = kb.alias(junk2, junk2.t[:].bitcast(I32))
        e0 = ckvE[0].t[:].rearrange("p a b -> p (a b)")
        e1 = ckvE[1].t[:].rearrange("p a b -> p (a b)")
        k0f = kpeT[0].t[:].bitcast(F32)
        cg = [kb.alias(ckvT[i], ckvT[i].t[:, 0:1280].rearrange("p (g c) -> p g c", g=G)) for i in range(2)]
        cg += [kb.alias(kpeT[i], kpeT[i].t[:, 512:1792].rearrange("p (g c) -> p g c", g=G)) for i in range(2)]
        x1v = xin[1].t[:].bitcast(BF16)
        cg.append(kb.alias(xin[1], x1v[:, 0:1280].rearrange("p (g c) -> p g c", g=G)))
        NCG = len(cg)
        pTs = [kb.alias(ckvT[i], ckvT[i].t[:, 1280:1792]) for i in range(2)]
        kp4 = kb.alias(ckvE[0], e0[:, 0:1024].rearrange("p (g a r) -> p g a r", g=G, a=4))
        pTn = kb.alias(ckvE[0], e0[:, 1024:1088])
        cpT = kb.alias(ckvE[1], e1[:, 0:1024])
        kpT = kb.alias(ckvE[1], e1[:, 1024:2048])
        rds = kb.alias(kpeT[0], k0f[:, 0:64])
        pcol = kb.alias(kpeT[0], k0f[:, 64:65])
        smask = kb.alias(kpeT[1], kpeT[1].t[:, 0:128].rearrange("p (b t) -> p b t", b=DB))
        kb.dma(pool, lambda: pool.e.dma_start(out=smask[:], in_=smask_d), writes=[smask])
        kb.dma(sp, lambda: sp.e.dma_start(out=pcol[:], in_=pcol_d), writes=[pcol])
        kb.dma(sp, lambda: sp.e.dma_start(out=ptab_i[:], in_=ptab_d.partition_broadcast(128)), writes=[ptab_i])
        kb.op(dve, lambda: dve.e.tensor_copy(out=idx_f[:], in_=ptab_i[:]), reads=[ptab_i], writes=[idx_f])
        kb.op(dve, lambda: dve.e.tensor_scalar(out=idx_f[:], in0=idx_f[:], scalar1=128.0, scalar2=pcol[:, 0:1],
                                               op0=ALU.mult, op1=ALU.add), reads=[idx_f, pcol], writes=[idx_f])
        kb.op(dve, lambda: dve.e.tensor_copy(out=idx_i[:], in_=idx_f[:]), reads=[idx_f], writes=[idx_i])
        for b in range(DB):
            qs = slice(b * DT, (b + 1) * DT)
            nb = NPAGE // G
            for bi in range(nb):
                i2 = (b * nb + bi) % 2
                ic = (b * nb + bi) % NCG
                for g in range(G):
                    col = b * NPAGE + bi * G + g
                    kb.dma(pool, lambda g=g, col=col, ic=ic: pool.e.indirect_dma_start(
                        out=cg[ic][:, g, :], out_offset=None, in_=cache_d[:, :],
                        in_offset=bass.IndirectOffsetOnAxis(ap=idx_i[:, col:col + 1], axis=0)),
                        reads=[idx_i], writes=[cg[ic]])
                kb.op(dve, lambda ic=ic: dve.e.tensor_copy(
                    out=kp4[:], in_=cg[ic][:, :, KVL:KVL + ROPE].unsqueeze(2).to_broadcast([128, G, 4, 32])),
                    reads=[cg[ic]], writes=[kp4])
                kb.group(pe, [lambda g=g, ic=ic: pe.e.transpose(
                    out=psb(PS[0], g * 128, (g + 1) * 128), in_=cg[ic][:, g, 0:KVL], identity=identb[:])
                    for g in range(G)], reads=[cg[ic], identb], writes=[PS[0]])
                kb.op(act, lambda: act.e.activation(out=cpT[:], in_=psb(PS[0], 0, G * 128), func=AF.Copy),
                      reads=[PS[0]], writes=[cpT])
                kb.group(pe, [lambda g=g: pe.e.transpose(
                    out=psb(PS[1], g * 128, (g + 1) * 128), in_=kp4[:, g, :, :].rearrange("p a r -> p (a r)"),
                    identity=identb[:]) for g in range(G)], reads=[kp4, identb], writes=[PS[1]])
                kb.op(dve, lambda: dve.e.tensor_copy(out=kpT[:], in_=psb(PS[1], 0, G * 128)),
                      reads=[PS[1]], writes=[kpT])
                bank = PS[2 + i2]
                fns = []
                for g in range(G):
                    fns.append(lambda g=g, bank=bank: pe.e.matmul(
                        bank[:, g * 64:(g + 1) * 64], lhsT=cpT[:, g * 128:(g + 1) * 128], rhs=q_latT[:, :, qs],
                        start=True, stop=False))
                    fns.append(lambda g=g, bank=bank: pe.e.matmul(
                        bank[:, g * 64:(g + 1) * 64], lhsT=kpT[:, g * 128:(g + 1) * 128], rhs=qTm[:, :, qs],
                        start=False, stop=True))
                kb.group(pe, fns, reads=[cpT, kpT, q_latT, qTm], writes=[bank])
                kb.op(act, lambda bank=bank, i2=i2: act.e.activation(out=pTs[i2][:], in_=bank[:, 0:G * 64],
                                                                   func=AF.Exp, scale=SCALE),
                      reads=[bank], writes=[pTs[i2]])
                fns = []
                for g in range(G):
                    first = (bi == 0 and g == 0)
                    fns.append(lambda g=g, i2=i2, ic=ic, first=first: pe.e.matmul(
                        PS[4][:, 0:64], lhsT=cg[ic][:, g, 0:KVL], rhs=pTs[i2][:, g * 64:(g + 1) * 64],
                        start=first, stop=False))
                    fns.append(lambda g=g, i2=i2, first=first: pe.e.matmul(
                        PS[5][:, 0:64], lhsT=ones_b[:, :], rhs=pTs[i2][:, g * 64:(g + 1) * 64],
                        start=first, stop=False))
                kb.group(pe, fns, reads=[cg[ic], pTs[i2], ones_b], writes=[PS[4], PS[5]])
            p6 = PS[6]
            kb.group(pe, [lambda: pe.e.matmul(p6[:, 0:64], lhsT=ckvT_s[:, :], rhs=q_latT[:, :, qs],
                                             start=True, stop=False),
                          lambda: pe.e.matmul(p6[:, 0:64], lhsT=kpeT_s[:, :], rhs=qTm[:, :, qs],
                                             start=False, stop=True)],
                     reads=[ckvT_s, kpeT_s, q_latT, qTm], writes=[p6])
            kb.op(act, lambda: act.e.activation(out=pTn[:], in_=p6[:, 0:64], func=AF.Exp, scale=SCALE),
                  reads=[p6], writes=[pTn])
            kb.op(dve, lambda b=b: dve.e.tensor_tensor(
                out=pTn[:, :].rearrange("p (h t) -> p h t", h=NH), in0=pTn[:, :].rearrange("p (h t) -> p h t", h=NH),
                in1=smask[:, b, :].unsqueeze(1).to_broadcast([128, NH, DT]), op=ALU.mult),
                reads=[pTn, smask], writes=[pTn])
            kb.group(pe, [lambda: pe.e.matmul(PS[4][:, 0:64], lhsT=ckvE_s[:, :], rhs=pTn[:, :], start=False, stop=True),
                          lambda: pe.e.matmul(PS[5][:, 0:64], lhsT=ones_b[:, :], rhs=pTn[:, :], start=False, stop=True)],
                     reads=[ckvE_s, pTn, ones_b], writes=[PS[4], PS[5]])
            kb.op(dve, lambda: dve.e.reciprocal(out=rds[:], in_=PS[5][:, 0:64]), reads=[PS[5]], writes=[rds])
            kb.op(dve, lambda: dve.e.tensor_tensor(
                out=o_latT[:, :, qs], in0=PS[4][:, 0:64].rearrange("p (h t) -> p h t", h=NH),
                in1=rds[:, :].rearrange("p (h t) -> p h t", h=NH), op=ALU.mult),
                reads=[PS[4], rds], writes=[o_latT])

    def phase_c(t):
        xi = xin[t % 2]
        p0 = PS[0]
        for j in range(4):
            kb.group(pe, [lambda j=j, h2=h2: pe.e.matmul(
                p0[:, j * 128:(j + 1) * 128], lhsT=w_uv[:, 2 * j + h2, :],
                rhs=o_latT[:, 2 * j + h2, :], start=(h2 == 0), stop=(h2 == 1)) for h2 in range(2)],
                reads=[w_uv, o_latT], writes=[p0])
        kb.op(act, lambda: act.e.activation(out=mixT[:, 4:8, :], in_=p0[:, :].rearrange("p (k n) -> p k n", k=4),
                                            func=AF.Copy), reads=[p0], writes=[mixT])
        kb.op(act, lambda: act.e.activation(out=osq[:], in_=p0[:, :], func=AF.Square), reads=[p0], writes=[osq])
        kb.group(pe, [lambda j=j: pe.e.matmul(PS[1][:, 0:2], lhsT=osq[:, j * 128:(j + 1) * 128], rhs=ones_f[:, 0:2],
                                             start=(j == 0), stop=(j == 3)) for j in range(4)],
                 reads=[osq, ones_f], writes=[PS[1]])
        kb.op(dve, lambda: dve.e.tensor_copy(out=stat2[:, 1:2], in_=PS[1][:, 0:1]), reads=[PS[1]], writes=[stat2])
        kb.op(dve, lambda: dve.e.tensor_scalar(out=stat2[:, 2:4], in0=stat2[:, 0:2], scalar1=1.0 / 512,
                                               scalar2=NORM_EPS, op0=ALU.mult, op1=ALU.add),
              reads=[stat2], writes=[stat2])
        kb.op(act, lambda: act.e.activation(out=stat2[:, 4:6], in_=stat2[:, 2:4], func=AF.Sqrt),
              reads=[stat2], writes=[stat2])
        kb.op(dve, lambda: dve.e.reciprocal(out=stat2[:, 6:8], in_=stat2[:, 4:6]), reads=[stat2], writes=[stat2])
        for g in range(2):
            for half in range(2):
                kb.group(pe, [lambda k=k, g=g, half=half: pe.e.matmul(
                    PS[2 + 2 * g + half][:, 0:512], lhsT=mixT[:, 4 * g + k, :],
                    rhs=w_o[:, 4 * g + k, half * 512:(half + 1) * 512], start=(k == 0), stop=(k == 3))
                    for k in range(4)], reads=[mixT, w_o], writes=[PS[2 + 2 * g + half]])
        for half in range(2):
            hs = slice(half * 512, (half + 1) * 512)
            kb.op(dve, lambda half=half, hs=hs: dve.e.tensor_scalar(out=pre[:, hs], in0=PS[2 + half][:, 0:512],
                                                                   scalar1=stat2[:, 6:7], scalar2=None, op0=ALU.mult),
                  reads=[PS[2 + half], stat2], writes=[pre])
            kb.op(dve, lambda half=half, hs=hs: dve.e.scalar_tensor_tensor(
                out=pre[:, hs], in0=PS[4 + half][:, 0:512], scalar=stat2[:, 7:8], in1=pre[:, hs],
                op0=ALU.mult, op1=ALU.add), reads=[PS[4 + half], stat2, pre], writes=[pre])
        kb.op(dve, lambda: dve.e.scalar_tensor_tensor(out=pre[:], in0=xi[:], scalar=float(ALPHA), in1=pre[:],
                                                      op0=ALU.mult, op1=ALU.add), reads=[xi, pre], writes=[pre])
        layer_norm(pre, x1, ln1g, ln1b)

    def layer_norm(src, dst, g, b):
        kb.op(act, lambda: act.e.activation(out=junk2[:], in_=src[:], func=AF.Copy, accum_out=stat2[:, 8:9]),
              reads=[src], writes=[junk2, stat2])
        kb.op(act, lambda: act.e.activation(out=junk2[:], in_=src[:], func=AF.Square, accum_out=stat2[:, 9:10]),
              reads=[src], writes=[junk2, stat2])
        kb.op(dve, lambda: dve.e.tensor_scalar(out=stat2[:, 10:12], in0=stat2[:, 8:10], scalar1=1.0 / D,
                                               scalar2=None, op0=ALU.mult), reads=[stat2], writes=[stat2])
        kb.op(dve, lambda: dve.e.tensor_tensor(out=stat2[:, 12:13], in0=stat2[:, 10:11], in1=stat2[:, 10:11],
                                               op=ALU.mult), reads=[stat2], writes=[stat2])
        kb.op(dve, lambda: dve.e.tensor_tensor(out=stat2[:, 13:14], in0=stat2[:, 11:12], in1=stat2[:, 12:13],
                                               op=ALU.subtract), reads=[stat2], writes=[stat2])
        kb.op(dve, lambda: dve.e.tensor_scalar(out=stat2[:, 13:14], in0=stat2[:, 13:14], scalar1=LN_EPS,
                                               scalar2=None, op0=ALU.add), reads=[stat2], writes=[stat2])
        kb.op(act, lambda: act.e.activation(out=stat2[:, 14:15], in_=stat2[:, 13:14], func=AF.Sqrt),
              reads=[stat2], writes=[stat2])
        kb.op(dve, lambda: dve.e.reciprocal(out=stat2[:, 15:16], in_=stat2[:, 14:15]), reads=[stat2], writes=[stat2])
        kb.op(dve, lambda: dve.e.tensor_scalar(out=dst[:], in0=src[:], scalar1=stat2[:, 10:11],
                                               scalar2=stat2[:, 15:16], op0=ALU.subtract, op1=ALU.mult),
              reads=[src, stat2], writes=[dst])
        kb.op(dve, lambda: dve.e.tensor_tensor(out=dst[:], in0=dst[:], in1=g[:], op=ALU.mult),
              reads=[dst, g], writes=[dst])
        kb.op(dve, lambda: dve.e.tensor_tensor(out=dst[:], in0=dst[:], in1=b[:], op=ALU.add),
              reads=[dst, b], writes=[dst])


    w_router = kb.sb([128, 8, NE], BF16, "w_router")
    ws_gate = kb.sb([128, 8, 256], BF16, "ws_gate")
    ws_up = kb.sb([128, 8, 256], BF16, "ws_up")
    ws_down = kb.sb([128, 2, D], BF16, "ws_down")
    rbias = kb.sb([128, NE], F32, "rbias")
    ebase = kb.sb([128, NE], F32, "ebase")
    dummycol = kb.sb([128, 1], F32, "dummycol")
    ustrict = kb.sb([128, 128], BF16, "ustrict")
    tokid = kb.sb([128, NT, 16], I32, "tokid")
    ln2g = kb.sb([128, D], F32, "ln2g")
    ln2b = kb.sb([128, D], F32, "ln2b")
    x1b = kb.sb([128, D], BF16, "x1b")
    x1T = kb.sb([128, 8, 128], BF16, "x1T")
    sg = kb.sb([128, 512], F32, "sg")
    hTs = kb.sb([128, 2, 128], BF16, "hTs")
    pre2 = kb.sb([128, D], F32, "pre2t")
    r_s = kb.sb([128, NE], F32, "r_s")
    r_sb = kb.sb([128, NE], F32, "r_sb")
    r_sbm = kb.sb([128, NE], F32, "r_sbm")
    r_gtop = kb.sb([128, 8, 8], F32, "r_gtop")
    r_sm = kb.sb([128, 64], F32, "r_sm")
    r_M = kb.sb([128, NE], BF16, "r_M")
    r_Msum = kb.sb([128, NE], BF16, "r_Msum")
    r_W = kb.sb([128, NE], F32, "r_W")
    r_v = kb.sb([128, NE], F32, "r_v")
    r_lt = kb.sb([128, NE], F32, "r_lt")
    r_junk = kb.sb([128, NE], F32, "r_junk")
    destI = kb.sb([128, NT, 8], I32, "destI")
    w8 = kb.sb([128, NT, 8], F32, "w8")
    zrow = pT[0]
    for k in range(8):
        kb.dma(pool, lambda k=k: pool.e.dma_start(out=w_router[:, k, :], in_=w_router_d[k * 128:(k + 1) * 128, :]),
               writes=[w_router])
        kb.dma(pool, lambda k=k: pool.e.dma_start(out=ws_gate[:, k, :], in_=ws_gate_d[k * 128:(k + 1) * 128, :]),
               writes=[ws_gate])
        kb.dma(pool, lambda k=k: pool.e.dma_start(out=ws_up[:, k, :], in_=ws_up_d[k * 128:(k + 1) * 128, :]),
               writes=[ws_up])
    for c in range(2):
        kb.dma(pool, lambda c=c: pool.e.dma_start(out=ws_down[:, c, :], in_=ws_down_d[c * 128:(c + 1) * 128, :]),
               writes=[ws_down])
    kb.dma(sp, lambda: sp.e.dma_start(out=rbias[:], in_=rbias_d.partition_broadcast(128)), writes=[rbias])
    kb.dma(sp, lambda: sp.e.dma_start(out=ebase[:], in_=ebase_d.partition_broadcast(128)), writes=[ebase])
    kb.dma(sp, lambda: sp.e.dma_start(out=dummycol[:], in_=dummycol_d), writes=[dummycol])
    kb.dma(pool, lambda: pool.e.dma_start(out=ustrict[:], in_=ustrict_d), writes=[ustrict])
    kb.dma(sp, lambda: sp.e.dma_start(out=tokid[:], in_=tokid_d), writes=[tokid])
    kb.dma(sp, lambda: sp.e.dma_start(out=ln2g[:], in_=ln2g_d.partition_broadcast(128)), writes=[ln2g])
    kb.dma(sp, lambda: sp.e.dma_start(out=ln2b[:], in_=ln2b_d.partition_broadcast(128)), writes=[ln2b])
    fillv = junk2.t[:].bitcast(I32)
    kb.op(dve, lambda: dve.e.memset(fillv, NTOK), writes=[junk2])
    kb.op(dve, lambda: dve.e.memset(zrow[:], 0.0), writes=[zrow])
    kb.op(dve, lambda: dve.e.memset(r_Msum[:], 0.0), writes=[r_Msum])
    st_flat = slot_tok_d.rearrange("(p r) c -> p (r c)", p=128)
    for a in range(8):
        kb.dma(sp, lambda a=a: sp.e.dma_start(out=st_flat[:, a * 1024:(a + 1) * 1024], in_=fillv),
               reads=[junk2], writes=[slot_tok_b])
    kb.dma(sp, lambda: sp.e.dma_start(out=st_flat[:, 8192:8208], in_=fillv[:, 0:16]), reads=[junk2], writes=[slot_tok_b])
    kb.dma(sp, lambda: sp.e.dma_start(out=x1tab_d[NTOK:NTOK + 128, :], in_=zrow[:]), reads=[zrow], writes=[x1tab_b])
    kb.dma(sp, lambda: sp.e.dma_start(out=yslots_d[NSLOT:NSLOT + 128, :], in_=zrow[:]), reads=[zrow], writes=[yslots_b])

    def phase_moe1(t):
        kb.op(act, lambda: act.e.activation(out=x1b[:], in_=x1[:], func=AF.Copy), reads=[x1], writes=[x1b])
        kb.dma(sp, lambda: sp.e.dma_start(out=x1tab_d[t * 128:(t + 1) * 128, :], in_=x1b[:]),
               reads=[x1b], writes=[x1tab_b])
        p6 = PS[6]
        kb.group(pe, [lambda k=k: pe.e.transpose(out=psb(p6, k * 128, (k + 1) * 128),
                                                in_=x1b[:, k * 128:(k + 1) * 128], identity=identb[:])
                      for k in range(8)], reads=[x1b, identb], writes=[p6])
        kb.op(act, lambda: act.e.activation(out=x1T[:], in_=psb(p6, 0, 1024).rearrange("p (k n) -> p k n", k=8),
                                            func=AF.Copy), reads=[p6], writes=[x1T])
        p7 = PS[7]
        kb.group(pe, [lambda k=k: pe.e.matmul(p7[:, 0:NE], lhsT=x1T[:, k, :], rhs=w_router[:, k, :],
                                             start=(k == 0), stop=(k == 7)) for k in range(8)],
                 reads=[x1T, w_router], writes=[p7])
        p0 = PS[0]
        for m, wm in enumerate((ws_gate, ws_up)):
            for c in range(2):
                kb.group(pe, [lambda k=k, m=m, c=c, wm=wm: pe.e.matmul(
                    p0[:, (m * 2 + c) * 128:(m * 2 + c + 1) * 128], lhsT=wm[:, k, c * 128:(c + 1) * 128],
                    rhs=x1T[:, k, :], start=(k == 0), stop=(k == 7)) for k in range(8)],
                    reads=[wm, x1T], writes=[p0])
        kb.op(act, lambda: act.e.activation(out=sg[:, 0:256], in_=p0[:, 0:256], func=AF.Silu),
              reads=[p0], writes=[sg])
        kb.op(dve, lambda: dve.e.tensor_tensor(out=hTs[:].rearrange("p c n -> p (c n)"), in0=sg[:, 0:256],
                                               in1=p0[:, 256:512], op=ALU.mult), reads=[sg, p0], writes=[hTs])
        for half in range(2):
            kb.group(pe, [lambda c=c, half=half: pe.e.matmul(
                PS[2 + half][:, 0:512], lhsT=hTs[:, c, :], rhs=ws_down[:, c, half * 512:(half + 1) * 512],
                start=(c == 0), stop=(c == 1)) for c in range(2)], reads=[hTs, ws_down], writes=[PS[2 + half]])
            kb.op(dve, lambda half=half: dve.e.scalar_tensor_tensor(
                out=pre2[:, half * 512:(half + 1) * 512], in0=x1[:, half * 512:(half + 1) * 512],
                scalar=float(ALPHA), in1=PS[2 + half][:, 0:512], op0=ALU.mult, op1=ALU.add),
                reads=[x1, PS[2 + half]], writes=[pre2])
        kb.dma(sp, lambda: sp.e.dma_start(out=pre2_d[t * 128:(t + 1) * 128, :], in_=pre2[:]),
               reads=[pre2], writes=[pre2_b])
        kb.op(act, lambda: act.e.activation(out=r_s[:], in_=p7[:, 0:NE], func=AF.Sigmoid), reads=[p7], writes=[r_s])
        kb.op(dve, lambda: dve.e.tensor_tensor(out=r_sb[:], in0=r_s[:], in1=rbias[:], op=ALU.add),
              reads=[r_s, rbias], writes=[r_sb])
        for g in range(8):
            kb.op(dve, lambda g=g: dve.e.max(out=r_gtop[:, g, :], in_=r_sb[:, g * 32:(g + 1) * 32]),
                  reads=[r_sb], writes=[r_gtop])
        kb.op(dve, lambda: dve.e.tensor_tensor(out=r_sm[:, 0:8], in0=r_gtop[:, :, 0], in1=r_gtop[:, :, 1], op=ALU.add),
              reads=[r_gtop], writes=[r_sm])
        kb.op(dve, lambda: dve.e.max(out=r_sm[:, 8:16], in_=r_sm[:, 0:8]), reads=[r_sm], writes=[r_sm])
        kb.op(dve, lambda: dve.e.tensor_scalar(out=r_sm[:, 16:24], in0=r_sm[:, 0:8], scalar1=r_sm[:, 11:12],
                                               scalar2=None, op0=ALU.is_ge), reads=[r_sm], writes=[r_sm])
        kb.op(dve, lambda: dve.e.tensor_scalar(out=r_sm[:, 24:32], in0=r_sm[:, 16:24], scalar1=-1.0, scalar2=1e30,
                                               op0=ALU.add, op1=ALU.mult), reads=[r_sm], writes=[r_sm])
        v3 = lambda b: b[:, :].rearrange("p (g e) -> p g e", g=8)
        kb.op(dve, lambda: dve.e.tensor_tensor(out=v3(r_sbm), in0=v3(r_sb),
                                               in1=r_sm[:, 16:24].unsqueeze(2).to_broadcast([128, 8, 32]),
                                               op=ALU.mult), reads=[r_sb, r_sm], writes=[r_sbm])
        kb.op(dve, lambda: dve.e.tensor_tensor(out=v3(r_sbm), in0=v3(r_sbm),
                                               in1=r_sm[:, 24:32].unsqueeze(2).to_broadcast([128, 8, 32]),
                                               op=ALU.add), reads=[r_sbm, r_sm], writes=[r_sbm])
        kb.op(dve, lambda: dve.e.max(out=r_sm[:, 32:40], in_=r_sbm[:]), reads=[r_sbm], writes=[r_sm])
        kb.op(dve, lambda: dve.e.tensor_scalar(out=r_M[:], in0=r_sbm[:], scalar1=r_sm[:, 39:40], scalar2=None,
                                               op0=ALU.is_ge), reads=[r_sbm, r_sm], writes=[r_M])
        kb.op(dve, lambda: dve.e.tensor_tensor(out=r_W[:], in0=r_s[:], in1=r_M[:], op=ALU.mult),
              reads=[r_s, r_M], writes=[r_W])
        kb.op(dve, lambda: dve.e.tensor_reduce(out=r_sm[:, 48:49], in_=r_W[:], axis=AX.X, op=ALU.add),
              reads=[r_W], writes=[r_sm])
        kb.op(dve, lambda: dve.e.reciprocal(out=r_sm[:, 49:50], in_=r_sm[:, 48:49]), reads=[r_sm], writes=[r_sm])
        kb.op(dve, lambda: dve.e.tensor_scalar(out=r_W[:], in0=r_W[:], scalar1=r_sm[:, 49:50],
                                               scalar2=float(ROUTED_SCALE), op0=ALU.mult, op1=ALU.mult),
              reads=[r_W, r_sm], writes=[r_W])
        p1 = PS[1]
        kb.group(pe, [lambda: pe.e.matmul(p1[:, 0:NE], lhsT=ustrict[:], rhs=r_M[:], start=True, stop=False),
                      lambda: pe.e.matmul(p1[:, 0:NE], lhsT=ones_b[:], rhs=r_Msum[:], start=False, stop=True)],
                 reads=[ustrict, r_M, ones_b, r_Msum], writes=[p1])
        kb.op(dve, lambda: dve.e.tensor_tensor(out=r_Msum[:], in0=r_Msum[:], in1=r_M[:], op=ALU.add),
              reads=[r_Msum, r_M], writes=[r_Msum])
        kb.op(dve, lambda: dve.e.tensor_tensor(out=r_v[:], in0=p1[:, 0:NE], in1=ebase[:], op=ALU.add),
              reads=[p1, ebase], writes=[r_v])
        kb.op(dve, lambda: dve.e.tensor_scalar(out=r_lt[:], in0=p1[:, 0:NE], scalar1=float(CAP), scalar2=None,
                                               op0=ALU.is_lt), reads=[p1], writes=[r_lt])
        kb.op(dve, lambda: dve.e.tensor_tensor(out=r_lt[:], in0=r_lt[:], in1=r_M[:], op=ALU.mult),
              reads=[r_lt, r_M], writes=[r_lt])
        kb.op(dve, lambda: dve.e.tensor_tensor(out=r_v[:], in0=r_v[:], in1=r_lt[:], op=ALU.mult),
              reads=[r_v, r_lt], writes=[r_v])
        kb.op(dve, lambda: dve.e.max(out=r_sm[:, 40:48], in_=r_v[:]), reads=[r_v], writes=[r_sm])
        for k in range(8):
            kb.op(dve, lambda k=k: dve.e.scalar_tensor_tensor(
                out=r_junk[:], in0=r_v[:], scalar=r_sm[:, 40 + k:41 + k], in1=r_W[:], op0=ALU.is_equal,
                op1=ALU.mult, accum_out=w8[:, t, k:k + 1]), reads=[r_v, r_sm, r_W], writes=[r_junk, w8])
        kb.op(dve, lambda: dve.e.tensor_scalar(out=r_gtop[:, 0, :], in0=r_sm[:, 40:48], scalar1=0.0, scalar2=None,
                                               op0=ALU.is_equal), reads=[r_sm], writes=[r_gtop])
        kb.op(dve, lambda: dve.e.scalar_tensor_tensor(out=r_gtop[:, 1, :], in0=r_gtop[:, 0, :], scalar=dummycol[:, 0:1],
                                                      in1=r_sm[:, 40:48], op0=ALU.mult, op1=ALU.add),
              reads=[r_gtop, dummycol, r_sm], writes=[r_gtop])
        kb.op(dve, lambda: dve.e.tensor_scalar(out=destI[:, t, :], in0=r_gtop[:, 1, :], scalar1=-1.0, scalar2=None,
                                               op0=ALU.add), reads=[r_gtop], writes=[destI])
        for k in range(8):
            kb.dma(pool, lambda k=k: pool.e.indirect_dma_start(
                out=slot_tok_d[:, :], out_offset=bass.IndirectOffsetOnAxis(ap=destI[:, t, k:k + 1], axis=0),
                in_=tokid[:, t, :], in_offset=None), reads=[destI, tokid], writes=[slot_tok_b])

    ids_sb = [kb.sb([128, 2, 16], I32, f"ids{i}") for i in range(2)]
    hT = kb.sb([128, 2, 256], BF16, "hT")

    def alloc_moe_aliases():
        nonlocal xg, xgT, wg, wu, wd, yb, acc, yg, outt
        xg = [kb.alias(xin[i], xin[i].t[:].bitcast(BF16).rearrange("p (j d) -> p j d", j=2)) for i in range(2)]
        wflat = w_in.t[:].rearrange("p k c -> p (k c)")
        wv = lambda n: wflat[:, n * 2048:(n + 1) * 2048]
        wg = [kb.alias(w_in, wv(3 * i + 0).rearrange("p (k f) -> p k f", k=8)) for i in range(2)]
        wu = [kb.alias(w_in, wv(3 * i + 1).rearrange("p (k f) -> p k f", k=8)) for i in range(2)]
        wd = [kb.alias(w_in, wv(3 * i + 2).rearrange("p (c d) -> p c d", c=2)) for i in range(2)]
        yb = [kb.alias(ckvT[i], ckvT[i].t[:].rearrange("p (j d) -> p j d", j=2)) for i in range(2)]
        xgT = kb.alias(ckvE[0], ckvE[0].t[:].rearrange("p a b -> p (a b)").rearrange("p (k n) -> p k n", k=8))
        acc = kb.alias(kpeT[0], kpeT[0].t[:].bitcast(F32))
        outt = kb.alias(kpeT[1], kpeT[1].t[:].bitcast(F32))
        e1 = ckvE[1].t[:].rearrange("p a b -> p (a b)")
        yg = [kb.alias(ckvE[1], e1[:, 0:1024]), kb.alias(ckvE[1], e1[:, 1024:2048]),
              kb.alias(o_latT, o_latT.t[:].rearrange("p a b -> p (a b)")),
              kb.alias(q_latT, q_latT.t[:].rearrange("p a b -> p (a b)"))]

    xg = xgT = wg = wu = wd = yb = acc = yg = outt = None

    def moe_load(e):
        i = e % 2
        kb.dma(pool, lambda: pool.e.dma_start(out=wg[i][:], in_=w_gate_d[e].rearrange("p (k f) -> p k f", k=8)),
               writes=[wg[i]])
        kb.dma(pool, lambda: pool.e.dma_start(out=wu[i][:], in_=w_up_d[e].rearrange("p (k f) -> p k f", k=8)),
               writes=[wu[i]])
        kb.dma(pool, lambda: pool.e.dma_start(out=wd[i][:], in_=w_down_d[e].rearrange("p (c d) -> p c d", c=2)),
               writes=[wd[i]])
        kb.dma(sp, lambda: sp.e.dma_start(
            out=ids_sb[i][:], in_=slot_tok_d[e * CAP:(e + 1) * CAP, :].rearrange("(p j) c -> p j c", j=2)),
            reads=[slot_tok_b], writes=[ids_sb[i]])
        for j in range(2):
            kb.dma(pool, lambda j=j: pool.e.indirect_dma_start(
                out=xg[i][:, j, :], out_offset=None, in_=x1tab_d[:, :],
                in_offset=bass.IndirectOffsetOnAxis(ap=ids_sb[i][:, j, 0:1], axis=0)),
                reads=[ids_sb[i], x1tab_b], writes=[xg[i]])

    def moe_T(e):
        i = e % 2
        for j in range(2):
            pj = PS[j]
            kb.group(pe, [lambda k=k, j=j, pj=pj: pe.e.transpose(
                out=psb(pj, k * 128, (k + 1) * 128), in_=xg[i][:, j, k * 128:(k + 1) * 128], identity=identb[:])
                for k in range(8)], reads=[xg[i], identb], writes=[pj])
            if j == 0:
                kb.op(act, lambda j=j, pj=pj: act.e.activation(
                    out=xgT[:, :, j * 128:(j + 1) * 128], in_=psb(pj, 0, 1024).rearrange("p (k n) -> p k n", k=8),
                    func=AF.Copy), reads=[pj], writes=[xgT])
            else:
                kb.op(dve, lambda j=j, pj=pj: dve.e.tensor_copy(
                    out=xgT[:, :, j * 128:(j + 1) * 128], in_=psb(pj, 0, 1024).rearrange("p (k n) -> p k n", k=8)),
                    reads=[pj], writes=[xgT])

    def moe_GU(e):
        i = e % 2
        for m, wm in enumerate((wg[i], wu[i])):
            for c in range(2):
                kb.group(pe, [lambda k=k, m=m, c=c, wm=wm: pe.e.matmul(
                    PS[2 + m][:, c * 256:(c + 1) * 256], lhsT=wm[:, k, c * 128:(c + 1) * 128], rhs=xgT[:, k, :],
                    start=(k == 0), stop=(k == 7)) for k in range(8)], reads=[wm, xgT], writes=[PS[2 + m]])
        kb.op(act, lambda: act.e.activation(out=sg[:], in_=PS[2][:, :], func=AF.Silu), reads=[PS[2]], writes=[sg])
        kb.op(dve, lambda: dve.e.tensor_tensor(out=hT[:].rearrange("p c n -> p (c n)"), in0=sg[:], in1=PS[3][:, :],
                                               op=ALU.mult), reads=[sg, PS[3]], writes=[hT])

    def moe_D(e):
        i = e % 2
        for j in range(2):
            for half in range(2):
                pb = PS[4 + 2 * j + half]
                kb.group(pe, [lambda c=c, j=j, half=half, pb=pb: pe.e.matmul(
                    pb[:, 0:512], lhsT=hT[:, c, j * 128:(j + 1) * 128], rhs=wd[i][:, c, half * 512:(half + 1) * 512],
                    start=(c == 0), stop=(c == 1)) for c in range(2)], reads=[hT, wd[i]], writes=[pb])
                if half == 0:
                    kb.op(act, lambda j=j, half=half, pb=pb: act.e.activation(
                        out=yb[i][:, j, half * 512:(half + 1) * 512], in_=pb[:, 0:512], func=AF.Copy),
                        reads=[pb], writes=[yb[i]])
                else:
                    kb.op(dve, lambda j=j, half=half, pb=pb: dve.e.tensor_copy(
                        out=yb[i][:, j, half * 512:(half + 1) * 512], in_=pb[:, 0:512]),
                        reads=[pb], writes=[yb[i]])
        kb.dma(sp, lambda: sp.e.dma_start(
            out=yslots_d[e * CAP:(e + 1) * CAP, :].rearrange("(p j) d -> p j d", j=2), in_=yb[i][:]),
            reads=[yb[i]], writes=[yslots_b])

    def phase_moe3(t):
        kb.dma(sp, lambda: sp.e.dma_start(out=acc[:], in_=pre2_d[t * 128:(t + 1) * 128, :]),
               reads=[pre2_b], writes=[acc])
        for k in range(8):
            g = yg[k % 4]
            kb.dma(pool, lambda k=k, g=g: pool.e.indirect_dma_start(
                out=g[:], out_offset=None, in_=yslots_d[:, :],
                in_offset=bass.IndirectOffsetOnAxis(ap=destI[:, t, k:k + 1], axis=0)),
                reads=[destI, yslots_b], writes=[g])
            kb.op(dve, lambda k=k, g=g: dve.e.scalar_tensor_tensor(
                out=acc[:], in0=g[:], scalar=w8[:, t, k:k + 1], in1=acc[:], op0=ALU.mult, op1=ALU.add),
                reads=[g, w8, acc], writes=[acc])
        layer_norm(acc, outt, ln2g, ln2b)
        dst = y_s if t == NPT else y_p[t * 128:(t + 1) * 128, :]
        kb.dma(sp, lambda: sp.e.dma_start(out=dst, in_=outt[:]), reads=[outt])


    for t in (range(KNT) if KNT else range(NT)):
        phase_a(t)
        if STAGE >= 2 and t < NPT:
            if SUB >= 2:
                phase_b(t)
            if SUB >= 3:
                phase_c(t)
            if STAGE == 2 and SUB >= 3:
                kb.dma(sp, lambda t=t: sp.e.dma_start(out=y_p[t * 128:(t + 1) * 128, :], in_=x1[:]), reads=[x1])
            if STAGE >= 5:
                phase_moe1(t)
        if STAGE >= 3 and t == NPT:
            phase_bs()
            phase_c(t)
            if STAGE == 3:
                kb.dma(sp, lambda: sp.e.dma_start(out=y_s, in_=x1[:]), reads=[x1])
            if STAGE >= 5:
                phase_moe1(t)
    if STAGE >= 5:
        tiles = list(range(KNT) if KNT else range(NT))
        nexp = int(os.environ.get('KNE', str(NE)))
        alloc_moe_aliases()
        moe_load(0)
        if nexp > 1:
            moe_load(1)
        moe_T(0)
        moe_GU(0)
        for e in range(nexp):
            if e + 1 < nexp:
                moe_T(e + 1)
            moe_D(e)
            if e + 2 < nexp:
                moe_load(e + 2)
            if e + 1 < nexp:
                moe_GU(e + 1)
        for t in tiles:
            phase_moe3(t)

    if STAGE < 9:
        zt = kb.sb([128, D], F32, "zt")
        kb.op(dve, lambda: dve.e.memset(zt[:], 0.0), writes=[zt])
        if STAGE < 2 or KNT:
            for t in range(KNT if STAGE >= 2 else 0, NPT):
                kb.dma(sp, lambda t=t: sp.e.dma_start(out=y_p[t * 128:(t + 1) * 128, :], in_=zt[:]), reads=[zt])
        if STAGE < 3 or KNT:
            kb.dma(sp, lambda: sp.e.dma_start(out=y_s, in_=zt[:]), reads=[zt])

    kb.finish()
    nc._in_names = in_names
    return nc


_PROGRAM = None


def _get_program():
    global _PROGRAM
    if _PROGRAM is None:
        _PROGRAM = build_program()
    return _PROGRAM


def _rope_tables(pos):
    half = ROPE // 2
    inv = (np.float32(10000.0) ** (-np.arange(half, dtype=np.float32) / np.float32(half))).astype(np.float32)
    ang = pos.astype(np.float32)[:, None] * inv[None, :]
    return np.cos(ang).astype(np.float32), np.sin(ang).astype(np.float32)


def kernel(x_prompt, x_sample, cache_kv_latent, cache_k_rope, state_conv, page_table,
           w_in, conv_w, q_norm, w_uq, kv_norm, w_uk, w_uv, g_conv, g_attn, w_o,
           ln1_g, ln1_b, w_router, router_bias, w_gate, w_up, w_down,
           ws_gate, ws_up, ws_down, ln2_g, ln2_b):
    f = lambda a: np.ascontiguousarray(np.asarray(a), dtype=np.float32)
    x_prompt = f(x_prompt); x_sample = f(x_sample)
    w_uq0 = f(w_uq)[0].reshape(QL, NH, NOPE + ROPE)
    w_uq_p = np.ascontiguousarray(np.concatenate(
        [w_uq0[:, :, :NOPE].reshape(QL, NH * NOPE), w_uq0[:, :, NOPE:].reshape(QL, NH * ROPE)], axis=1))
    w_uk0 = f(w_uk)[0]
    w_ukT = np.zeros((128, NH, KVL), np.float32)
    w_uvz = np.zeros((KVL, NH, 128), np.float32)
    w_uv0 = f(w_uv)[0]
    for h in range(NH):
        w_ukT[(h % 2) * NOPE:(h % 2 + 1) * NOPE, h, :] = w_uk0[:, h, :].T
        w_uvz[:, h, (h % 2) * VD:(h % 2 + 1) * VD] = w_uv0[:, h, :]
    rowmask = np.zeros((128, 4), np.float32)
    for i in range(4):
        rowmask[i * 32:(i + 1) * 32, i] = 1.0
    gmix = np.ascontiguousarray(np.concatenate([f(g_conv)[0], f(g_attn)[0]]).reshape(8, 128).T)
    qn = np.ascontiguousarray(f(q_norm)[0].reshape(2, 128).T)
    cw = np.ascontiguousarray(f(conv_w)[0].reshape(3, 4, 128).transpose(2, 1, 0))
    cos_p, sin_p = _rope_tables(np.arange(SEQ))
    cos_p = np.ascontiguousarray(cos_p.reshape(NQB, 128, 16).transpose(1, 0, 2))
    sin_p = np.ascontiguousarray(sin_p.reshape(NQB, 128, 16).transpose(1, 0, 2))
    cos_s, sin_s = _rope_tables(PAST + (np.arange(128) % DT))
    ident = np.eye(128, dtype=np.float32)
    cmask = (np.arange(128)[:, None] <= np.arange(128)[None, :]).astype(np.float32)
    ustrict = (np.arange(128)[:, None] < np.arange(128)[None, :]).astype(np.float32)
    ebase = (np.arange(NE, dtype=np.float32) * CAP + 1.0).reshape(1, NE)
    dummycol = (NSLOT + 1 + np.arange(128, dtype=np.float32)).reshape(128, 1)
    tokid = np.ascontiguousarray(np.broadcast_to(
        (np.arange(NT, dtype=np.int32)[None, :, None] * 128 + np.arange(128, dtype=np.int32)[:, None, None]),
        (128, NT, 16)))
    common = {
        "ustrict": ustrict, "ebase": ebase, "dummycol": dummycol, "tokid": tokid,
        "w_in": f(w_in)[0], "w_uq": w_uq_p, "w_ukT": w_ukT, "w_uv": w_uvz, "rowmask": rowmask,
        "w_o": f(w_o)[0], "gmix": gmix, "qnorm": qn, "kvnorm": f(kv_norm)[0].reshape(1, KVL), "convw": cw,
        "ln1g": f(ln1_g)[0].reshape(1, D), "ln1b": f(ln1_b)[0].reshape(1, D),
        "ln2g": f(ln2_g)[0].reshape(1, D), "ln2b": f(ln2_b)[0].reshape(1, D),
        "w_router": f(w_router)[0], "rbias": f(router_bias)[0].reshape(1, NE),
        "ws_gate": f(ws_gate)[0], "ws_up": f(ws_up)[0], "ws_down": f(ws_down)[0],
        "w_gate": np.ascontiguousarray(f(w_gate)[0].reshape(NE, 8, 128, 256).transpose(0, 2, 1, 3)).reshape(NE, 128, 2048),
        "w_up": np.ascontiguousarray(f(w_up)[0].reshape(NE, 8, 128, 256).transpose(0, 2, 1, 3)).reshape(NE, 128, 2048),
        "w_down": np.ascontiguousarray(f(w_down)[0].reshape(NE, 2, 128, D).transpose(0, 2, 1, 3)).reshape(NE, 128, 2048),
        "cache_all": np.ascontiguousarray(np.concatenate(
            [f(cache_kv_latent)[0].reshape(NPOOL * 128, KVL), f(cache_k_rope)[0].reshape(NPOOL * 128, ROPE)], axis=1)),
        "cosp": cos_p, "sinp": sin_p, "coss": cos_s, "sins": sin_s, "identf": ident, "cmask": cmask,
    }
    smask = np.zeros((128, DB, DT), np.float32)
    for b in range(DB):
        for tq in range(DT):
            smask[b * DT:b * DT + tq + 1, b, tq] = 1.0
    common["smask"] = smask
    common["pcol"] = np.arange(128, dtype=np.float32).reshape(128, 1)
    pt = np.ascontiguousarray(np.asarray(page_table), dtype=np.int32)
    sc = f(state_conv)[0]
    in_maps = []
    for c in range(NCORES):
        m = dict(common)
        m["xp"] = x_prompt[NSEQ * c:NSEQ * (c + 1)].reshape(NPT * 128, D)
        m["xs"] = x_sample[DB * c:DB * (c + 1)].reshape(128, D)
        m["ptab"] = pt[DB * c:DB * (c + 1)].reshape(1, DB * NPAGE)
        m["sconv"] = sc[DB * c:DB * (c + 1)]
        in_maps.append(m)
    nc = _get_program()
    in_maps = [{k: v for k, v in m.items() if k in nc._in_names} for m in in_maps]
    kne = int(os.environ.get('KNE', str(NE)))
    if kne != NE:
        for m in in_maps:
            for k in ("w_gate", "w_up", "w_down"):
                m[k] = m[k][:kne]
    res = run_bass_kernel_spmd(nc, in_maps, core_ids=list(range(NCORES)))
    R = res.results
    cat = lambda k: np.concatenate([np.asarray(R[c][k]) for c in range(NCORES)], axis=0)
    y_prompt = cat("y_p").reshape(16, SEQ, D)
    y_sample = cat("y_s").reshape(128, DT, D)
    p_kv = cat("kv_p").reshape(1, 16, SEQ, KVL)
    p_kr = cat("kr_p").reshape(1, 16, SEQ, ROPE)
    p_cs = cat("cs_p").reshape(1, 16, 2, CONV_CH)
    s_kv = cat("kv_s").reshape(1, 128, DT, KVL)
    s_kr = cat("kr_s").reshape(1, 128, DT, ROPE)
    s_cs = cat("cs_s").reshape(1, 128, 2, CONV_CH)
    return (y_prompt, y_sample, p_kv, p_kr, p_cs, s_kv, s_kr, s_cs)
```

```python
import numpy as np
import ml_dtypes
import concourse.bass as bass
import concourse.mybir as mybir
from concourse.bass_utils import run_bass_kernel_spmd

F32 = mybir.dt.float32
BF16 = mybir.dt.bfloat16
I32 = mybir.dt.int32
AF = mybir.ActivationFunctionType
ALU = mybir.AluOpType
AX = mybir.AxisListType

NCORES = 8
D = 1024
SEQ = 2048
NSEQ = 2
NQB = SEQ // 128
NPT = NSEQ * NQB
NT = NPT + 1
NTOK = NT * 128
DB = 16
DT = 8
PAST = 8192
NPAGE = 64
NPOOL = 10240
CONV_CH = 512
QL = 256
KVL = 128
ROPE = 32
NH = 8
NOPE = 64
VD = 64
INC = 1952
NE = 256
CAP = 256
NSLOT = NE * CAP
ALPHA = 2.0 ** 0.25
SCALE = (NOPE + ROPE) ** -0.5
NORM_EPS = 1e-6
LN_EPS = 1e-5
ROUTED_SCALE = 2.5

import os
STAGE = int(os.environ.get('KSTAGE', '5'))
SUB = int(os.environ.get('KSUB', '9'))
KNT = int(os.environ.get('KNT', '0'))
KDBG = int(os.environ.get('KDBG', '99'))
NPOOLD = NPOOL


class Eng:
    def __init__(self, nc, name, e):
        self.e = e
        self.name = name
        self.sem = nc.alloc_semaphore(name + "_prog")
        self.cnt = 0
        self.waited = {}


class Buf:
    def __init__(self, t):
        self.t = t
        self.w = None
        self.r = []

    def __getitem__(self, k):
        return self.t[k]


class KB:
    def __init__(self, nc):
        self.nc = nc
        self.pe = Eng(nc, "pe", nc.tensor)
        self.act = Eng(nc, "act", nc.scalar)
        self.dve = Eng(nc, "dve", nc.vector)
        self.pool = Eng(nc, "pool", nc.gpsimd)
        self.sp = Eng(nc, "sp", nc.sync)
        self.dma_sems = [[nc.alloc_semaphore(f"dma{i}"), 0] for i in range(40)]
        self.dma_rr = 0
        self.nbuf = 0

    def sb(self, shape, dt, name=None):
        self.nbuf += 1
        return Buf(self.nc.alloc_sbuf_tensor("s_" + (name or f"sb{self.nbuf}"), list(shape), dt))

    def ps(self, name=None):
        self.nbuf += 1
        return Buf(self.nc.alloc_psum_tensor("p_" + (name or f"ps{self.nbuf}"), [128, 512], F32))

    def alias(self, parent, view):
        b = Buf(view)
        b.r = list(parent.r)
        if parent.w is not None:
            b.r.append(parent.w)
        for ch in getattr(parent, "children", []):
            b.r.extend(ch.r)
            if ch.w is not None:
                b.r.append(ch.w)
        if not hasattr(parent, "children"):
            parent.children = []
        parent.children.append(b)
        return b

    def need(self, X, deps):
        for d in deps:
            if d is None:
                continue
            sem, val = d
            if sem is X.sem and X.name == "pe":
                continue
            key = id(sem)
            if X.waited.get(key, 0) < val:
                X.e.wait_ge(sem, val)
                X.waited[key] = val

    def _deps(self, reads, writes, extra):
        deps = list(extra)
        for b in reads:
            if b.w is not None:
                deps.append(b.w)
        for b in writes:
            if b.w is not None:
                deps.append(b.w)
            deps.extend(b.r)
        return deps

    def _commit(self, tok, reads, writes):
        for b in reads:
            b.r.append(tok)
            if len(b.r) > 24:
                b.r = b.r[-24:]
        for b in writes:
            b.w = tok
            b.r = []

    def op(self, X, fn, reads=(), writes=(), extra=()):
        self.need(X, self._deps(reads, writes, extra))
        inst = fn()
        X.cnt += 1
        inst.then_inc(X.sem, 1)
        tok = (X.sem, X.cnt)
        self._commit(tok, reads, writes)
        return tok

    def group(self, X, fns, reads=(), writes=(), extra=()):
        self.need(X, self._deps(reads, writes, extra))
        inst = None
        for fn in fns:
            inst = fn()
        X.cnt += 1
        inst.then_inc(X.sem, 1)
        tok = (X.sem, X.cnt)
        self._commit(tok, reads, writes)
        return tok

    def dma(self, Q, fn, reads=(), writes=(), extra=()):
        slot = self.dma_sems[self.dma_rr % len(self.dma_sems)]
        self.dma_rr += 1
        deps = self._deps(reads, writes, extra)
        if slot[1] > 0:
            deps.append((slot[0], slot[1]))
        self.need(Q, deps)
        inst = fn()
        slot[1] += 16
        inst.then_inc(slot[0], 16)
        tok = (slot[0], slot[1])
        self._commit(tok, reads, writes)
        return tok

    def finish(self):
        deps = [(s, v) for s, v in self.dma_sems if v > 0]
        for E in (self.pe, self.act, self.dve, self.pool):
            if E.cnt > 0:
                deps.append((E.sem, E.cnt))
        self.need(self.sp, deps)


def build_program():
    nc = bass.Bass("TRN2", target_bir_lowering=False)
    kb = KB(nc)
    pe, act, dve, pool, sp = kb.pe, kb.act, kb.dve, kb.pool, kb.sp

    in_names = []

    def din(name, shape, dt=F32):
        in_names.append(name)
        return nc.dram_tensor(name, list(shape), dt, kind="ExternalInput").ap()

    def dout(name, shape, dt=F32):
        return nc.dram_tensor(name, list(shape), dt, kind="ExternalOutput").ap()

    xp = din("xp", [NPT * 128, D])
    xs = din("xs", [128, D])
    w_in_d = din("w_in", [D, INC])
    w_uq_d = din("w_uq", [QL, 768])
    w_ukT_d = din("w_ukT", [128, 8, 128])
    w_uv_d = din("w_uv", [128, 8, 128])
    rowmask_d = din("rowmask", [128, 4])
    w_o_d = din("w_o", [D, D])
    gmix_d = din("gmix", [128, 8])
    qnorm_d = din("qnorm", [128, 2])
    kvnorm_d = din("kvnorm", [1, 128])
    convw_d = din("convw", [128, 4, 3])
    ln1g_d = din("ln1g", [1, D])
    ln1b_d = din("ln1b", [1, D])
    ln2g_d = din("ln2g", [1, D])
    ln2b_d = din("ln2b", [1, D])
    w_router_d = din("w_router", [D, NE])
    rbias_d = din("rbias", [1, NE])
    ws_gate_d = din("ws_gate", [D, 256])
    ws_up_d = din("ws_up", [D, 256])
    ws_down_d = din("ws_down", [256, D])
    if STAGE >= 5:
        NED = int(os.environ.get('KNE', str(NE)))
        w_gate_d = din("w_gate", [NED, 128, 2048])
        w_up_d = din("w_up", [NED, 128, 2048])
        w_down_d = din("w_down", [NED, 128, 2048])
    if STAGE >= 3:
        cache_d = din("cache_all", [NPOOLD * 128, KVL + ROPE])
        smask_d = din("smask", [128, DB, DT])
        pcol_d = din("pcol", [128, 1])
    ptab_d = din("ptab", [1, DB * NPAGE], I32)
    sconv_d = din("sconv", [DB, 2, CONV_CH])
    cosp_d = din("cosp", [128, NQB, 16])
    sinp_d = din("sinp", [128, NQB, 16])
    coss_d = din("coss", [128, 16])
    sins_d = din("sins", [128, 16])
    identf_d = din("identf", [128, 128])
    cmask_d = din("cmask", [128, 128])

    ustrict_d = din("ustrict", [128, 128])
    ebase_d = din("ebase", [1, NE])
    dummycol_d = din("dummycol", [128, 1])
    tokid_d = din("tokid", [128, NT, 16], I32)
    x1tab_d = nc.dram_tensor("x1tab", [NTOK + 128, D], BF16).ap()
    pre2_d = nc.dram_tensor("pre2", [NTOK, D], F32).ap()
    slot_tok_d = nc.dram_tensor("slot_tok", [NSLOT + 128, 16], I32).ap()
    yslots_d = nc.dram_tensor("yslots", [NSLOT + 128, D], BF16).ap()
    x1tab_b = Buf(x1tab_d); pre2_b = Buf(pre2_d); slot_tok_b = Buf(slot_tok_d); yslots_b = Buf(yslots_d)

    y_p = dout("y_p", [NPT * 128, D])
    y_s = dout("y_s", [128, D])
    kv_p = dout("kv_p", [NPT * 128, KVL])
    kr_p = dout("kr_p", [NPT * 128, ROPE])
    cs_p = dout("cs_p", [NSEQ, 2, CONV_CH])
    kv_s = dout("kv_s", [128, KVL])
    kr_s = dout("kr_s", [128, ROPE])
    cs_s = dout("cs_s", [DB, 2, CONV_CH])

    identf = kb.sb([128, 128], F32, "identf")
    identb = kb.sb([128, 128], BF16, "identb")
    ones_f = kb.sb([128, 128], F32, "ones_f")
    ones_b = kb.sb([128, 128], BF16, "ones_b")
    cmask = kb.sb([128, 128], BF16, "cmask")
    w_in = kb.sb([128, 8, INC], BF16, "w_in")
    w_uq = kb.sb([128, 2, 768], BF16, "w_uq")
    w_ukT = kb.sb([128, 8, 128], BF16, "w_ukT")
    w_uv = kb.sb([128, 8, 128], BF16, "w_uv")
    rowmask = kb.sb([128, 4], F32, "rowmask")
    qTm = kb.sb([128, 8, 128], BF16, "qTm")
    qnorm = kb.sb([128, 2], F32, "qnorm")
    kvnorm = kb.sb([128, 128], F32, "kvnorm")
    convw = kb.sb([128, 4, 3], F32, "convw")
    cosp = kb.sb([128, NQB, 16], F32, "cosp")
    sinp = kb.sb([128, NQB, 16], F32, "sinp")
    coss = kb.sb([128, 16], F32, "coss")
    sins = kb.sb([128, 16], F32, "sins")

    kb.dma(sp, lambda: sp.e.dma_start(out=identf[:], in_=identf_d), writes=[identf])
    kb.dma(pool, lambda: pool.e.dma_start(out=identb[:], in_=identf_d), writes=[identb])
    kb.dma(pool, lambda: pool.e.dma_start(out=cmask[:], in_=cmask_d), writes=[cmask])
    kb.op(dve, lambda: dve.e.memset(ones_f[:], 1.0), writes=[ones_f])
    kb.op(dve, lambda: dve.e.memset(ones_b[:], 1.0), writes=[ones_b])
    for k in range(8):
        kb.dma(pool, lambda k=k: pool.e.dma_start(out=w_in[:, k, :], in_=w_in_d[k * 128:(k + 1) * 128, :]),
               writes=[w_in])
    kb.dma(sp, lambda: sp.e.dma_start(out=qnorm[:], in_=qnorm_d), writes=[qnorm])
    kb.dma(pool, lambda: pool.e.dma_start(out=w_ukT[:], in_=w_ukT_d), writes=[w_ukT])
    kb.dma(pool, lambda: pool.e.dma_start(out=w_uv[:], in_=w_uv_d), writes=[w_uv])
    kb.dma(sp, lambda: sp.e.dma_start(out=rowmask[:], in_=rowmask_d), writes=[rowmask])
    kb.dma(sp, lambda: sp.e.dma_start(out=kvnorm[:], in_=kvnorm_d.partition_broadcast(128)), writes=[kvnorm])
    kb.dma(sp, lambda: sp.e.dma_start(out=convw[:], in_=convw_d), writes=[convw])
    kb.dma(sp, lambda: sp.e.dma_start(out=cosp[:], in_=cosp_d), writes=[cosp])
    kb.dma(sp, lambda: sp.e.dma_start(out=sinp[:], in_=sinp_d), writes=[sinp])
    kb.dma(sp, lambda: sp.e.dma_start(out=coss[:], in_=coss_d), writes=[coss])
    kb.dma(sp, lambda: sp.e.dma_start(out=sins[:], in_=sins_d), writes=[sins])

    PS = [kb.ps(f"psb{i}") for i in range(8)]
    xin = [kb.sb([128, D], F32, f"xin{i}") for i in range(2)]
    xT = kb.sb([128, 8, 128], BF16, "xT")
    ctmp = kb.sb([128, 128], F32, "ctmp")
    uT = kb.sb([128, 4, 130], F32, "uT")
    uS = kb.sb([128, 4, DB, 10], F32, "uS")
    ytmp = kb.sb([128, 128], F32, "ytmp")
    zf = kb.sb([128, 128], F32, "zf")
    zsq = kb.sb([128, 128], F32, "zsq")
    mixT = kb.sb([128, 8, 128], BF16, "mixT")
    stat = kb.sb([128, 8], F32, "stat")
    junk = kb.sb([128, 512], F32, "junk")
    ckvn = kb.sb([128, 128], F32, "ckvn")
    ckvb = kb.sb([128, 128], BF16, "ckvb")
    kr = kb.sb([128, 32], F32, "kr")
    rt = kb.sb([128, 4, 16], F32, "rt")
    kr4 = kb.sb([128, 128], BF16, "kr4")
    ckvT = [kb.sb([128, SEQ], BF16, f"ckvT{s}") for s in range(NSEQ)]
    ckvE = [kb.sb([128, NQB, 128], BF16, f"ckvE{s}") for s in range(NSEQ)]
    kpeT = [kb.sb([128, SEQ], BF16, f"kpeT{s}") for s in range(NSEQ)]
    ckvT_s = kb.sb([128, 128], BF16, "ckvT_s")
    ckvE_s = kb.sb([128, 128], BF16, "ckvE_s")
    kpeT_s = kb.sb([128, 128], BF16, "kpeT_s")

    qa_bf = kb.sb([128, 256], BF16, "qa_bf")
    qaT = kb.sb([128, 2, 128], BF16, "qaT")
    q_sb = kb.sb([128, 768], F32, "q_sb")
    q_bf = kb.sb([128, 768], BF16, "q_bf")
    qrt = kb.sb([128, 4, 8, 16], F32, "qrt")
    qT = kb.sb([128, 6, 128], BF16, "qT")
    q_latT = kb.sb([128, 8, 128], BF16, "q_latT")
    pT = [kb.sb([128, 1024], BF16, f"pT{i}") for i in range(2)]
    rden = kb.sb([128, 1024], F32, "rden")
    o_latT = kb.sb([128, 8, 128], BF16, "o_latT")
    osq = kb.sb([128, 512], F32, "osq")
    stat2 = kb.sb([128, 16], F32, "stat2")
    pre = kb.sb([128, D], F32, "pre")
    x1 = kb.sb([128, D], F32, "x1")
    w_o = kb.sb([128, 8, D], BF16, "w_o")
    w_o_f = pre
    junk2 = kb.sb([128, D], F32, "junk2")
    for k in range(2):
        kb.dma(sp, lambda k=k: sp.e.dma_start(out=junk2[:, 0:768], in_=w_uq_d[k * 128:(k + 1) * 128, :]),
               writes=[junk2])
        kb.op(dve, lambda k=k: dve.e.tensor_scalar(out=w_uq[:, k, :], in0=junk2[:, 0:768],
                                                   scalar1=qnorm[:, k:k + 1], scalar2=None, op0=ALU.mult),
              reads=[junk2, qnorm], writes=[w_uq])
    gmix = kb.sb([128, 8], F32, "gmix")
    ln1g = kb.sb([128, D], F32, "ln1g")
    ln1b = kb.sb([128, D], F32, "ln1b")
    kb.dma(sp, lambda: sp.e.dma_start(out=gmix[:], in_=gmix_d), writes=[gmix])
    kb.dma(sp, lambda: sp.e.dma_start(out=ln1g[:], in_=ln1g_d.partition_broadcast(128)), writes=[ln1g])
    kb.dma(sp, lambda: sp.e.dma_start(out=ln1b[:], in_=ln1b_d.partition_broadcast(128)), writes=[ln1b])
    for k in range(8):
        kb.dma(sp, lambda k=k: sp.e.dma_start(out=w_o_f[:], in_=w_o_d[k * 128:(k + 1) * 128, :]), writes=[w_o_f])
        kb.op(dve, lambda k=k: dve.e.tensor_scalar(out=w_o[:, k, :], in0=w_o_f[:], scalar1=gmix[:, k:k + 1],
                                                   scalar2=None, op0=ALU.mult),
              reads=[w_o_f, gmix], writes=[w_o])

    def psb(buf, lo, hi):
        return buf.t[:].bitcast(BF16)[:, lo:hi]

    def phase_a(t):
        sample = (t == NPT)
        s, qb = (t // NQB, t % NQB) if not sample else (0, 0)
        xi = xin[t % 2]
        src = xs if sample else xp[t * 128:(t + 1) * 128, :]
        kb.dma(sp, lambda: sp.e.dma_start(out=xi[:], in_=src), writes=[xi])
        for half in range(2):
            pb = PS[half]
            kb.group(pe, [lambda k=k, pb=pb, half=half: pe.e.transpose(
                out=pb[:, (k - 4 * half) * 128:(k - 4 * half + 1) * 128],
                in_=xi[:, k * 128:(k + 1) * 128], identity=identf[:]) for k in range(4 * half, 4 * half + 4)],
                reads=[xi, identf], writes=[pb])
            kb.op(act, lambda pb=pb, half=half: act.e.activation(
                out=xT[:, 4 * half:4 * half + 4, :], in_=pb[:, :].rearrange("p (k n) -> p k n", k=4),
                func=AF.Copy), reads=[pb], writes=[xT])
        pq = PS[2]
        kb.group(pe, [lambda k=k: pe.e.matmul(pq[:, 0:416], lhsT=xT[:, k, :], rhs=w_in[:, k, 1536:1952],
                                             start=(k == 0), stop=(k == 7)) for k in range(8)],
                 reads=[xT, w_in], writes=[pq])
        cw = convw
        for j in range(4):
            pc = PS[3 + (j % 2)]
            for g in range(3):
                col0 = g * 512 + j * 128
                kb.group(pe, [lambda k=k, g=g, col0=col0, pc=pc: pe.e.matmul(
                    pc[:, g * 128:(g + 1) * 128], lhsT=w_in[:, k, col0:col0 + 128], rhs=xT[:, k, :],
                    start=(k == 0), stop=(k == 7)) for k in range(8)],
                    reads=[xT, w_in], writes=[pc])
            kb.op(act, lambda pc=pc: act.e.activation(out=ctmp[:], in_=pc[:, 128:256], func=AF.Copy),
                  reads=[pc], writes=[ctmp])
            if not sample:
                if qb == 0:
                    kb.op(dve, lambda j=j: dve.e.memset(uT[:, j, 0:2], 0.0), writes=[uT])
                kb.op(dve, lambda j=j, pc=pc: dve.e.tensor_tensor(out=uT[:, j, 2:130], in0=ctmp[:],
                                                                 in1=pc[:, 256:384], op=ALU.mult),
                      reads=[ctmp, pc], writes=[uT])
                kb.op(dve, lambda j=j: dve.e.tensor_scalar(out=ytmp[:], in0=uT[:, j, 0:128],
                                                           scalar1=cw[:, j, 0:1], scalar2=None, op0=ALU.mult),
                      reads=[uT, cw], writes=[ytmp])
                for i in (1, 2):
                    kb.op(dve, lambda j=j, i=i: dve.e.scalar_tensor_tensor(
                        out=ytmp[:], in0=uT[:, j, i:i + 128], scalar=cw[:, j, i:i + 1], in1=ytmp[:],
                        op0=ALU.mult, op1=ALU.add), reads=[uT, cw, ytmp], writes=[ytmp])
                if qb == NQB - 1:
                    with nc.allow_non_contiguous_dma(reason="tiny conv-state store"):
                        kb.dma(sp, lambda j=j: sp.e.dma_start(
                            out=cs_p[s, :, j * 128:(j + 1) * 128].rearrange("i c -> c i"),
                            in_=uT[:, j, 128:130]), reads=[uT])
                else:
                    kb.op(dve, lambda j=j: dve.e.tensor_copy(out=uT[:, j, 0:2], in_=uT[:, j, 128:130]),
                          reads=[uT], writes=[uT])
            else:
                with nc.allow_non_contiguous_dma(reason="tiny conv-state load"):
                    for i in range(2):
                        kb.dma(sp, lambda j=j, i=i: sp.e.dma_start(
                            out=uS[:, j, :, i:i + 1],
                            in_=sconv_d[:, i, j * 128:(j + 1) * 128].rearrange("b (c o) -> c b o", o=1)),
                            writes=[uS])
                kb.op(dve, lambda j=j, pc=pc: dve.e.tensor_tensor(
                    out=uS[:, j, :, 2:10], in0=ctmp[:].rearrange("p (b t) -> p b t", t=DT),
                    in1=pc[:, 256:384].rearrange("p (b t) -> p b t", t=DT), op=ALU.mult),
                    reads=[ctmp, pc], writes=[uS])
                yv = ytmp[:].rearrange("p (b t) -> p b t", t=DT)
                kb.op(dve, lambda j=j: dve.e.tensor_scalar(out=yv, in0=uS[:, j, :, 0:8],
                                                           scalar1=cw[:, j, 0:1], scalar2=None, op0=ALU.mult),
                      reads=[uS, cw], writes=[ytmp])
                for i in (1, 2):
                    kb.op(dve, lambda j=j, i=i: dve.e.scalar_tensor_tensor(
                        out=yv, in0=uS[:, j, :, i:i + 8], scalar=cw[:, j, i:i + 1], in1=yv,
                        op0=ALU.mult, op1=ALU.add), reads=[uS, cw, ytmp], writes=[ytmp])
                with nc.allow_non_contiguous_dma(reason="tiny conv-state store"):
                    for i in range(2):
                        kb.dma(sp, lambda j=j, i=i: sp.e.dma_start(
                            out=cs_s[:, i, j * 128:(j + 1) * 128].rearrange("b (c o) -> c b o", o=1),
                            in_=uS[:, j, :, 8 + i:9 + i]), reads=[uS])
            kb.op(dve, lambda pc=pc: dve.e.tensor_tensor(out=zf[:], in0=ytmp[:], in1=pc[:, 0:128], op=ALU.mult),
                  reads=[ytmp, pc], writes=[zf])
            kb.op(act, lambda j=j: act.e.activation(out=mixT[:, j, :], in_=zf[:], func=AF.Copy),
                  reads=[zf], writes=[mixT])
            kb.op(act, lambda: act.e.activation(out=zsq[:], in_=zf[:], func=AF.Square),
                  reads=[zf], writes=[zsq])
            kb.group(pe, [lambda j=j: pe.e.matmul(PS[5][:, 0:2], lhsT=zsq[:], rhs=ones_f[:, 0:2],
                                                 start=(j == 0), stop=(j == 3))],
                     reads=[zsq, ones_f], writes=[PS[5]])
        kb.op(dve, lambda: dve.e.tensor_copy(out=stat2[:, 0:1], in_=PS[5][:, 0:1]), reads=[PS[5]], writes=[stat2])
        kb.op(act, lambda: act.e.activation(out=junk[:, 0:256], in_=pq[:, 0:256], func=AF.Square,
                                            accum_out=stat[:, 0:1]), reads=[pq], writes=[junk, stat])
        kb.op(act, lambda: act.e.activation(out=junk[:, 256:384], in_=pq[:, 256:384], func=AF.Square,
                                            accum_out=stat[:, 1:2]), reads=[pq], writes=[junk, stat])
        kb.op(dve, lambda: dve.e.tensor_scalar(out=stat[:, 2:3], in0=stat[:, 0:1], scalar1=1.0 / QL,
                                               scalar2=NORM_EPS, op0=ALU.mult, op1=ALU.add),
              reads=[stat], writes=[stat])
        kb.op(dve, lambda: dve.e.tensor_scalar(out=stat[:, 3:4], in0=stat[:, 1:2], scalar1=1.0 / KVL,
                                               scalar2=NORM_EPS, op0=ALU.mult, op1=ALU.add),
              reads=[stat], writes=[stat])
        kb.op(act, lambda: act.e.activation(out=stat[:, 4:6], in_=stat[:, 2:4], func=AF.Sqrt),
              reads=[stat], writes=[stat])
        kb.op(dve, lambda: dve.e.reciprocal(out=stat[:, 6:8], in_=stat[:, 4:6]), reads=[stat], writes=[stat])
        kb.op(dve, lambda: dve.e.scalar_tensor_tensor(out=ckvn[:], in0=pq[:, 256:384], scalar=stat[:, 7:8],
                                                      in1=kvnorm[:], op0=ALU.mult, op1=ALU.mult),
              reads=[pq, stat, kvnorm], writes=[ckvn])
        kvo = kv_s if sample else kv_p[t * 128:(t + 1) * 128, :]
        kb.dma(sp, lambda: sp.e.dma_start(out=kvo, in_=ckvn[:]), reads=[ckvn])
        cE, cEv = (ckvE_s, ckvE_s[:, :]) if sample else (ckvE[s], ckvE[s][:, qb, :])
        kb.op(act, lambda: act.e.activation(out=cEv, in_=ckvn[:], func=AF.Copy), reads=[ckvn], writes=[cE])
        pt = PS[6]
        kb.group(pe, [lambda: pe.e.transpose(out=psb(pt, 0, 128), in_=cEv, identity=identb[:])],
                 reads=[cE, identb], writes=[pt])
        cT, cTv = (ckvT_s, ckvT_s[:, :]) if sample else (ckvT[s], ckvT[s][:, qb * 128:(qb + 1) * 128])
        kb.op(act, lambda: act.e.activation(out=cTv, in_=psb(pt, 0, 128), func=AF.Copy), reads=[pt], writes=[cT])
        cosv = coss[:, :] if sample else cosp[:, qb, :]
        sinv = sins[:, :] if sample else sinp[:, qb, :]
        tabs = [coss, sins] if sample else [cosp, sinp]
        x1 = pq[:, 384:400]
        x2 = pq[:, 400:416]
        kb.op(dve, lambda: dve.e.tensor_tensor(out=rt[:, 0, :], in0=x1, in1=cosv, op=ALU.mult),
              reads=[pq] + tabs, writes=[rt])
        kb.op(dve, lambda: dve.e.tensor_tensor(out=rt[:, 1, :], in0=x2, in1=sinv, op=ALU.mult),
              reads=[pq] + tabs, writes=[rt])
        kb.op(dve, lambda: dve.e.tensor_tensor(out=rt[:, 2, :], in0=x1, in1=sinv, op=ALU.mult),
              reads=[pq] + tabs, writes=[rt])
        kb.op(dve, lambda: dve.e.tensor_tensor(out=rt[:, 3, :], in0=x2, in1=cosv, op=ALU.mult),
              reads=[pq] + tabs, writes=[rt])
        kb.op(dve, lambda: dve.e.tensor_tensor(out=kr[:, 0:16], in0=rt[:, 0, :], in1=rt[:, 1, :], op=ALU.subtract),
              reads=[rt], writes=[kr])
        kb.op(dve, lambda: dve.e.tensor_tensor(out=kr[:, 16:32], in0=rt[:, 2, :], in1=rt[:, 3, :], op=ALU.add),
              reads=[rt], writes=[kr])
        kro = kr_s if sample else kr_p[t * 128:(t + 1) * 128, :]
        kb.dma(sp, lambda: sp.e.dma_start(out=kro, in_=kr[:]), reads=[kr])
        for rep in range(4):
            kb.op(act, lambda rep=rep: act.e.activation(out=kr4[:, rep * 32:(rep + 1) * 32], in_=kr[:],
                                                        func=AF.Copy), reads=[kr], writes=[kr4])
        kb.group(pe, [lambda: pe.e.transpose(out=psb(pt, 128, 256), in_=kr4[:], identity=identb[:])],
                 reads=[kr4, identb], writes=[pt])
        kT, kTv = (kpeT_s, kpeT_s[:, :]) if sample else (kpeT[s], kpeT[s][:, qb * 128:(qb + 1) * 128])
        kb.op(act, lambda: act.e.activation(out=kTv, in_=psb(pt, 128, 256), func=AF.Copy), reads=[pt], writes=[kT])
        if STAGE < 2:
            return
        if KDBG <= 0:
            return
        kb.op(act, lambda: act.e.activation(out=qa_bf[:], in_=pq[:, 0:256], func=AF.Copy), reads=[pq], writes=[qa_bf])
        p7 = PS[7]
        kb.group(pe, [lambda k=k: pe.e.transpose(out=psb(p7, k * 128, (k + 1) * 128),
                                                in_=qa_bf[:, k * 128:(k + 1) * 128], identity=identb[:])
                      for k in range(2)], reads=[qa_bf, identb], writes=[p7])
        kb.op(act, lambda: act.e.activation(out=qaT[:], in_=psb(p7, 0, 256).rearrange("p (k n) -> p k n", k=2),
                                            func=AF.Copy), reads=[p7], writes=[qaT])
        if KDBG <= 1:
            return
        kb.group(pe, [lambda k=k: pe.e.matmul(PS[0][:, 0:512], lhsT=qaT[:, k, :], rhs=w_uq[:, k, 0:512],
                                             start=(k == 0), stop=(k == 1)) for k in range(2)],
                 reads=[qaT, w_uq], writes=[PS[0]])
        kb.group(pe, [lambda k=k: pe.e.matmul(PS[1][:, 0:256], lhsT=qaT[:, k, :], rhs=w_uq[:, k, 512:768],
                                             start=(k == 0), stop=(k == 1)) for k in range(2)],
                 reads=[qaT, w_uq], writes=[PS[1]])
        kb.op(act, lambda: act.e.activation(out=q_bf[:, 0:512], in_=PS[0][:, 0:512], func=AF.Copy,
                                            scale=stat[:, 6:7]), reads=[PS[0], stat], writes=[q_bf])
        kb.op(act, lambda: act.e.activation(out=q_sb[:, 512:768], in_=PS[1][:, 0:256], func=AF.Copy,
                                            scale=stat[:, 6:7]), reads=[PS[1], stat], writes=[q_sb])
        if KDBG <= 2:
            return
        qv = q_sb[:, 512:768].rearrange("p (h r) -> p h r", h=NH)
        qo = q_bf[:, 512:768].rearrange("p (h r) -> p h r", h=NH)
        cb = cosv.unsqueeze(1).to_broadcast([128, NH, 16])
        sbv = sinv.unsqueeze(1).to_broadcast([128, NH, 16])
        qx1 = qv[:, :, 0:16]
        qx2 = qv[:, :, 16:32]
        kb.op(dve, lambda: dve.e.tensor_tensor(out=qrt[:, 0], in0=qx1, in1=cb, op=ALU.mult),
              reads=[q_sb] + tabs, writes=[qrt])
        kb.op(dve, lambda: dve.e.tensor_tensor(out=qrt[:, 1], in0=qx2, in1=sbv, op=ALU.mult),
              reads=[q_sb] + tabs, writes=[qrt])
        kb.op(dve, lambda: dve.e.tensor_tensor(out=qrt[:, 2], in0=qx1, in1=sbv, op=ALU.mult),
              reads=[q_sb] + tabs, writes=[qrt])
        kb.op(dve, lambda: dve.e.tensor_tensor(out=qrt[:, 3], in0=qx2, in1=cb, op=ALU.mult),
              reads=[q_sb] + tabs, writes=[qrt])
        kb.op(dve, lambda: dve.e.tensor_tensor(out=qo[:, :, 0:16], in0=qrt[:, 0], in1=qrt[:, 1], op=ALU.subtract),
              reads=[qrt], writes=[q_bf])
        kb.op(dve, lambda: dve.e.tensor_tensor(out=qo[:, :, 16:32], in0=qrt[:, 2], in1=qrt[:, 3], op=ALU.add),
              reads=[qrt], writes=[q_bf])
        if KDBG <= 3:
            return
        p3 = PS[3]
        kb.group(pe, [lambda k=k: pe.e.transpose(out=psb(p3, k * 128, (k + 1) * 128),
                                                in_=q_bf[:, k * 128:(k + 1) * 128], identity=identb[:])
                      for k in range(6)], reads=[q_bf, identb], writes=[p3])
        kb.op(act, lambda: act.e.activation(out=qT[:], in_=psb(p3, 0, 768).rearrange("p (k n) -> p k n", k=6),
                                            func=AF.Copy), reads=[p3], writes=[qT])
        if KDBG <= 4:
            return
        for half in range(2):
            pb = PS[half]
            for hh in range(4):
                h = 4 * half + hh
                kb.group(pe, [lambda h=h, hh=hh, pb=pb: pe.e.matmul(
                    pb[:, hh * 128:(hh + 1) * 128], lhsT=w_ukT[:, h, :],
                    rhs=qT[:, h // 2, :], start=True, stop=True)],
                    reads=[w_ukT, qT], writes=[pb])
            kb.op(act, lambda half=half, pb=pb: act.e.activation(
                out=q_latT[:, 4 * half:4 * half + 4, :], in_=pb[:, :].rearrange("p (k n) -> p k n", k=4),
                func=AF.Copy), reads=[pb], writes=[q_latT])
        for h in range(NH):
            kb.op(dve, lambda h=h: dve.e.tensor_scalar(out=qTm[:, h, :], in0=qT[:, 4 + h // 4, :],
                                                       scalar1=rowmask[:, h % 4:h % 4 + 1], scalar2=None,
                                                       op0=ALU.mult), reads=[qT, rowmask], writes=[qTm])

    def phase_b(t):
        s, qb = t // NQB, t % NQB
        for kb_i in range(qb + 1):
            bsel = kb_i % 2
            ks = slice(kb_i * 128, (kb_i + 1) * 128)
            for half in range(2):
                bank = PS[2 * bsel + half]
                fns = [lambda half=half, bank=bank, ks=ks: pe.e.matmul(
                    bank[:, 0:512], lhsT=ckvT[s][:, ks],
                    rhs=q_latT[:, 4 * half:4 * half + 4, :].rearrange("p k n -> p (k n)"),
                    start=True, stop=False)]
                for hh in range(4):
                    h = 4 * half + hh
                    fns.append(lambda hh=hh, h=h, bank=bank, ks=ks: pe.e.matmul(
                        bank[:, hh * 128:(hh + 1) * 128], lhsT=kpeT[s][:, ks],
                        rhs=qTm[:, h, :], start=False, stop=(hh == 3)))
                kb.group(pe, fns, reads=[ckvT[s], kpeT[s], q_latT, qTm], writes=[bank])
                kb.op(act, lambda half=half, bank=bank, bsel=bsel: act.e.activation(
                    out=pT[bsel][:, half * 512:(half + 1) * 512], in_=bank[:, 0:512], func=AF.Exp, scale=SCALE),
                    reads=[bank], writes=[pT[bsel]])
            if kb_i == qb:
                kb.op(dve, lambda bsel=bsel: dve.e.tensor_tensor(
                    out=pT[bsel][:, :].rearrange("p (h q) -> p h q", h=NH),
                    in0=pT[bsel][:, :].rearrange("p (h q) -> p h q", h=NH),
                    in1=cmask[:, :].unsqueeze(1).to_broadcast([128, NH, 128]), op=ALU.mult),
                    reads=[pT[bsel], cmask], writes=[pT[bsel]])
            for half in range(2):
                kb.group(pe, [lambda half=half, bsel=bsel, kb_i=kb_i: pe.e.matmul(
                    PS[4 + half][:, 0:512], lhsT=ckvE[s][:, kb_i, :], rhs=pT[bsel][:, half * 512:(half + 1) * 512],
                    start=(kb_i == 0), stop=(kb_i == qb))],
                    reads=[ckvE[s], pT[bsel]], writes=[PS[4 + half]])
                kb.group(pe, [lambda half=half, bsel=bsel, kb_i=kb_i: pe.e.matmul(
                    PS[6 + half][:, 0:512], lhsT=ones_b[:, :], rhs=pT[bsel][:, half * 512:(half + 1) * 512],
                    start=(kb_i == 0), stop=(kb_i == qb))],
                    reads=[ones_b, pT[bsel]], writes=[PS[6 + half]])
        for half in range(2):
            kb.op(dve, lambda half=half: dve.e.reciprocal(out=rden[:, half * 512:(half + 1) * 512],
                                                          in_=PS[6 + half][:, 0:512]),
                  reads=[PS[6 + half]], writes=[rden])
            kb.op(dve, lambda half=half: dve.e.tensor_tensor(
                out=o_latT[:, 4 * half:4 * half + 4, :].rearrange("p k n -> p (k n)"),
                in0=PS[4 + half][:, 0:512], in1=rden[:, half * 512:(half + 1) * 512], op=ALU.mult),
                reads=[PS[4 + half], rden], writes=[o_latT])


    def phase_bs():
        G = 8
        ptab_i = kb.alias(rden, rden.t[:].bitcast(I32))
        idx_f = pre
        idx_i = kb.alias(junk2, junk2.t[:].bitcast(I32))
        e0 = ckvE[0].t[:].rearrange("p a b -> p (a b)")
        e1 = ckvE[1].t[:].rearrange("p a b -> p (a b)")
        k0f = kpeT[0].t[:].bitcast(F32)
        cg = [kb.alias(ckvT[i], ckvT[i].t[:, 0:1280].rearrange("p (g c) -> p g c", g=G)) for i in range(2)]
        cg += [kb.alias(kpeT[i], kpeT[i].t[:, 512:1792].rearrange("p (g c) -> p g c", g=G)) for i in range(2)]
        x1v = xin[1].t[:].bitcast(BF16)
        cg.append(kb.alias(xin[1], x1v[:, 0:1280].rearrange("p (g c) -> p g c", g=G)))
        NCG = len(cg)
        pTs = [kb.alias(ckvT[i], ckvT[i].t[:, 1280:1792]) for i in range(2)]
        kp4s = [kb.alias(ckvE[0], e0[:, 0:1024].rearrange("p (g a r) -> p g a r", g=G, a=4)),
                kb.alias(pT[0], pT[0].t[:, 0:1024].rearrange("p (g a r) -> p g a r", g=G, a=4))]
        pTn = kb.alias(ckvE[0], e0[:, 1024:1088])
        cpTs = [kb.alias(ckvE[1], e1[:, 0:1024]), kb.alias(pT[1], pT[1].t[:, 0:1024])]
        kpTs = [kb.alias(ckvE[1], e1[:, 1024:2048]), kb.alias(q_sb, q_sb.t[:].bitcast(BF16)[:, 0:1024])]
        rds = kb.alias(kpeT[0], k0f[:, 0:64])
        pcol = kb.alias(kpeT[0], k0f[:, 64:65])
        smask = kb.alias(kpeT[1], kpeT[1].t[:, 0:128].rearrange("p (b t) -> p b t", b=DB))
        kb.dma(pool, lambda: pool.e.dma_start(out=smask[:], in_=smask_d), writes=[smask])
        kb.dma(sp, lambda: sp.e.dma_start(out=pcol[:], in_=pcol_d), writes=[pcol])
        kb.dma(sp, lambda: sp.e.dma_start(out=ptab_i[:], in_=ptab_d.partition_broadcast(128)), writes=[ptab_i])
        kb.op(dve, lambda: dve.e.tensor_copy(out=idx_f[:], in_=ptab_i[:]), reads=[ptab_i], writes=[idx_f])
        kb.op(dve, lambda: dve.e.tensor_scalar(out=idx_f[:], in0=idx_f[:], scalar1=128.0, scalar2=pcol[:, 0:1],
                                               op0=ALU.mult, op1=ALU.add), reads=[idx_f, pcol], writes=[idx_f])
        kb.op(dve, lambda: dve.e.tensor_copy(out=idx_i[:], in_=idx_f[:]), reads=[idx_f], writes=[idx_i])
        for b in range(DB):
            qs = slice(b * DT, (b + 1) * DT)
            nb = NPAGE // G
            for bi in range(nb):
                i2 = (b * nb + bi) % 2
                ic = (b * nb + bi) % NCG
                kp4, cpT, kpT = kp4s[i2], cpTs[i2], kpTs[i2]
                pA, pB = (PS[0], PS[1]) if i2 == 0 else (PS[7], PS[6])
                for g in range(G):
                    col = b * NPAGE + bi * G + g
                    kb.dma(pool, lambda g=g, col=col, ic=ic: pool.e.indirect_dma_start(
                        out=cg[ic][:, g, :], out_offset=None, in_=cache_d[:, :],
                        in_offset=bass.IndirectOffsetOnAxis(ap=idx_i[:, col:col + 1], axis=0)),
                        reads=[idx_i], writes=[cg[ic]])
                kb.op(dve, lambda ic=ic, kp4=kp4: dve.e.tensor_copy(
                    out=kp4[:], in_=cg[ic][:, :, KVL:KVL + ROPE].unsqueeze(2).to_broadcast([128, G, 4, 32])),
                    reads=[cg[ic]], writes=[kp4])
                kb.group(pe, [lambda g=g, ic=ic, pA=pA: pe.e.transpose(
                    out=psb(pA, g * 128, (g + 1) * 128), in_=cg[ic][:, g, 0:KVL], identity=identb[:])
                    for g in range(G)], reads=[cg[ic], identb], writes=[pA])
                kb.op(act, lambda cpT=cpT, pA=pA: act.e.activation(out=cpT[:], in_=psb(pA, 0, G * 128), func=AF.Copy),
                      reads=[pA], writes=[cpT])
                kb.group(pe, [lambda g=g, kp4=kp4, pB=pB: pe.e.transpose(
                    out=psb(pB, g * 128, (g + 1) * 128), in_=kp4[:, g, :, :].rearrange("p a r -> p (a r)"),
                    identity=identb[:]) for g in range(G)], reads=[kp4, identb], writes=[pB])
                kb.op(dve, lambda kpT=kpT, pB=pB: dve.e.tensor_copy(out=kpT[:], in_=psb(pB, 0, G * 128)),
                      reads=[pB], writes=[kpT])
                bank = PS[2 + i2]
                fns = []
                for g in range(G):
                    fns.append(lambda g=g, bank=bank, cpT=cpT: pe.e.matmul(
                        bank[:, g * 64:(g + 1) * 64], lhsT=cpT[:, g * 128:(g + 1) * 128], rhs=q_latT[:, :, qs],
                        start=True, stop=False))
                    fns.append(lambda g=g, bank=bank, kpT=kpT: pe.e.matmul(
                        bank[:, g * 64:(g + 1) * 64], lhsT=kpT[:, g * 128:(g + 1) * 128], rhs=qTm[:, :, qs],
                        start=False, stop=True))
                kb.group(pe, fns, reads=[cpT, kpT, q_latT, qTm], writes=[bank])
                kb.op(act, lambda bank=bank, i2=i2: act.e.activation(out=pTs[i2][:], in_=bank[:, 0:G * 64],
                                                                   func=AF.Exp, scale=SCALE),
                      reads=[bank], writes=[pTs[i2]])
                fns = []
                for g in range(G):
                    first = (bi == 0 and g == 0)
                    fns.append(lambda g=g, i2=i2, ic=ic, first=first: pe.e.matmul(
                        PS[4][:, 0:64], lhsT=cg[ic][:, g, 0:KVL], rhs=pTs[i2][:, g * 64:(g + 1) * 64],
                        start=first, stop=False))
                    fns.append(lambda g=g, i2=i2, first=first: pe.e.matmul(
                        PS[5][:, 0:64], lhsT=ones_b[:, :], rhs=pTs[i2][:, g * 64:(g + 1) * 64],
                        start=first, stop=False))
                kb.group(pe, fns, reads=[cg[ic], pTs[i2], ones_b], writes=[PS[4], PS[5]])
            p6 = PS[6]
            kb.group(pe, [lambda: pe.e.matmul(p6[:, 0:64], lhsT=ckvT_s[:, :], rhs=q_latT[:, :, qs],
                                             start=True, stop=False),
                          lambda: pe.e.matmul(p6[:, 0:64], lhsT=kpeT_s[:, :], rhs=qTm[:, :, qs],
                                             start=False, stop=True)],
                     reads=[ckvT_s, kpeT_s, q_latT, qTm], writes=[p6])
            kb.op(act, lambda: act.e.activation(out=pTn[:], in_=p6[:, 0:64], func=AF.Exp, scale=SCALE),
                  reads=[p6], writes=[pTn])
            kb.op(dve, lambda b=b: dve.e.tensor_tensor(
                out=pTn[:, :].rearrange("p (h t) -> p h t", h=NH), in0=pTn[:, :].rearrange("p (h t) -> p h t", h=NH),
                in1=smask[:, b, :].unsqueeze(1).to_broadcast([128, NH, DT]), op=ALU.mult),
                reads=[pTn, smask], writes=[pTn])
            kb.group(pe, [lambda: pe.e.matmul(PS[4][:, 0:64], lhsT=ckvE_s[:, :], rhs=pTn[:, :], start=False, stop=True),
                          lambda: pe.e.matmul(PS[5][:, 0:64], lhsT=ones_b[:, :], rhs=pTn[:, :], start=False, stop=True)],
                     reads=[ckvE_s, pTn, ones_b], writes=[PS[4], PS[5]])
            kb.op(dve, lambda: dve.e.reciprocal(out=rds[:], in_=PS[5][:, 0:64]), reads=[PS[5]], writes=[rds])
            kb.op(dve, lambda: dve.e.tensor_tensor(
                out=o_latT[:, :, qs], in0=PS[4][:, 0:64].rearrange("p (h t) -> p h t", h=NH),
                in1=rds[:, :].rearrange("p (h t) -> p h t", h=NH), op=ALU.mult),
                reads=[PS[4], rds], writes=[o_latT])

    def phase_c(t):
        xi = xin[t % 2]
        p0 = PS[0]
        for j in range(4):
            kb.group(pe, [lambda j=j, h2=h2: pe.e.matmul(
                p0[:, j * 128:(j + 1) * 128], lhsT=w_uv[:, 2 * j + h2, :],
                rhs=o_latT[:, 2 * j + h2, :], start=(h2 == 0), stop=(h2 == 1)) for h2 in range(2)],
                reads=[w_uv, o_latT], writes=[p0])
        kb.op(act, lambda: act.e.activation(out=mixT[:, 4:8, :], in_=p0[:, :].rearrange("p (k n) -> p k n", k=4),
                                            func=AF.Copy), reads=[p0], writes=[mixT])
        kb.op(act, lambda: act.e.activation(out=osq[:], in_=p0[:, :], func=AF.Square), reads=[p0], writes=[osq])
        kb.group(pe, [lambda j=j: pe.e.matmul(PS[1][:, 0:2], lhsT=osq[:, j * 128:(j + 1) * 128], rhs=ones_f[:, 0:2],
                                             start=(j == 0), stop=(j == 3)) for j in range(4)],
                 reads=[osq, ones_f], writes=[PS[1]])
        kb.op(dve, lambda: dve.e.tensor_copy(out=stat2[:, 1:2], in_=PS[1][:, 0:1]), reads=[PS[1]], writes=[stat2])
        kb.op(dve, lambda: dve.e.tensor_scalar(out=stat2[:, 2:4], in0=stat2[:, 0:2], scalar1=1.0 / 512,
                                               scalar2=NORM_EPS, op0=ALU.mult, op1=ALU.add),
              reads=[stat2], writes=[stat2])
        kb.op(act, lambda: act.e.activation(out=stat2[:, 4:6], in_=stat2[:, 2:4], func=AF.Sqrt),
              reads=[stat2], writes=[stat2])
        kb.op(dve, lambda: dve.e.reciprocal(out=stat2[:, 6:8], in_=stat2[:, 4:6]), reads=[stat2], writes=[stat2])
        for g in range(2):
            for half in range(2):
                kb.group(pe, [lambda k=k, g=g, half=half: pe.e.matmul(
                    PS[2 + 2 * g + half][:, 0:512], lhsT=mixT[:, 4 * g + k, :],
                    rhs=w_o[:, 4 * g + k, half * 512:(half + 1) * 512], start=(k == 0), stop=(k == 3))
                    for k in range(4)], reads=[mixT, w_o], writes=[PS[2 + 2 * g + half]])
        for half in range(2):
            hs = slice(half * 512, (half + 1) * 512)
            kb.op(dve, lambda half=half, hs=hs: dve.e.tensor_scalar(out=pre[:, hs], in0=PS[2 + half][:, 0:512],
                                                                   scalar1=stat2[:, 6:7], scalar2=None, op0=ALU.mult),
                  reads=[PS[2 + half], stat2], writes=[pre])
            kb.op(dve, lambda half=half, hs=hs: dve.e.scalar_tensor_tensor(
                out=pre[:, hs], in0=PS[4 + half][:, 0:512], scalar=stat2[:, 7:8], in1=pre[:, hs],
                op0=ALU.mult, op1=ALU.add), reads=[PS[4 + half], stat2, pre], writes=[pre])
        kb.op(dve, lambda: dve.e.scalar_tensor_tensor(out=pre[:], in0=xi[:], scalar=float(ALPHA), in1=pre[:],
                                                      op0=ALU.mult, op1=ALU.add), reads=[xi, pre], writes=[pre])
        layer_norm(pre, x1, ln1g, ln1b)

    def layer_norm(src, dst, g, b):
        kb.op(act, lambda: act.e.activation(out=junk2[:], in_=src[:], func=AF.Copy, accum_out=stat2[:, 8:9]),
              reads=[src], writes=[junk2, stat2])
        kb.op(act, lambda: act.e.activation(out=junk2[:], in_=src[:], func=AF.Square, accum_out=stat2[:, 9:10]),
              reads=[src], writes=[junk2, stat2])
        kb.op(dve, lambda: dve.e.tensor_scalar(out=stat2[:, 10:12], in0=stat2[:, 8:10], scalar1=1.0 / D,
                                               scalar2=None, op0=ALU.mult), reads=[stat2], writes=[stat2])
        kb.op(dve, lambda: dve.e.tensor_tensor(out=stat2[:, 12:13], in0=stat2[:, 10:11], in1=stat2[:, 10:11],
                                               op=ALU.mult), reads=[stat2], writes=[stat2])
        kb.op(dve, lambda: dve.e.tensor_tensor(out=stat2[:, 13:14], in0=stat2[:, 11:12], in1=stat2[:, 12:13],
                                               op=ALU.subtract), reads=[stat2], writes=[stat2])
        kb.op(dve, lambda: dve.e.tensor_scalar(out=stat2[:, 13:14], in0=stat2[:, 13:14], scalar1=LN_EPS,
                                               scalar2=None, op0=ALU.add), reads=[stat2], writes=[stat2])
        kb.op(act, lambda: act.e.activation(out=stat2[:, 14:15], in_=stat2[:, 13:14], func=AF.Sqrt),
              reads=[stat2], writes=[stat2])
        kb.op(dve, lambda: dve.e.reciprocal(out=stat2[:, 15:16], in_=stat2[:, 14:15]), reads=[stat2], writes=[stat2])
        kb.op(dve, lambda: dve.e.tensor_scalar(out=dst[:], in0=src[:], scalar1=stat2[:, 10:11],
                                               scalar2=stat2[:, 15:16], op0=ALU.subtract, op1=ALU.mult),
              reads=[src, stat2], writes=[dst])
        kb.op(dve, lambda: dve.e.tensor_tensor(out=dst[:], in0=dst[:], in1=g[:], op=ALU.mult),
              reads=[dst, g], writes=[dst])
        kb.op(dve, lambda: dve.e.tensor_tensor(out=dst[:], in0=dst[:], in1=b[:], op=ALU.add),
              reads=[dst, b], writes=[dst])


    w_router = kb.sb([128, 8, NE], BF16, "w_router")
    ws_gate = kb.sb([128, 8, 256], BF16, "ws_gate")
    ws_up = kb.sb([128, 8, 256], BF16, "ws_up")
    ws_down = kb.sb([128, 2, D], BF16, "ws_down")
    rbias = kb.sb([128, NE], F32, "rbias")
    ebase = kb.sb([128, NE], F32, "ebase")
    dummycol = kb.sb([128, 1], F32, "dummycol")
    ustrict = kb.sb([128, 128], BF16, "ustrict")
    tokid = kb.sb([128, NT, 16], I32, "tokid")
    ln2g = kb.sb([128, D], F32, "ln2g")
    ln2b = kb.sb([128, D], F32, "ln2b")
    x1b = kb.sb([128, D], BF16, "x1b")
    x1T = kb.sb([128, 8, 128], BF16, "x1T")
    sg = kb.sb([128, 512], F32, "sg")
    hTs = kb.sb([128, 2, 128], BF16, "hTs")
    pre2 = kb.sb([128, D], F32, "pre2t")
    r_s = kb.sb([128, NE], F32, "r_s")
    r_sb = kb.sb([128, NE], F32, "r_sb")
    r_sbm = kb.sb([128, NE], F32, "r_sbm")
    r_gtop = kb.sb([128, 8, 8], F32, "r_gtop")
    r_sm = kb.sb([128, 64], F32, "r_sm")
    r_M = kb.sb([128, NE], BF16, "r_M")
    r_Msum = kb.sb([128, NE], BF16, "r_Msum")
    r_W = kb.sb([128, NE], F32, "r_W")
    r_v = kb.sb([128, NE], F32, "r_v")
    r_lt = kb.sb([128, NE], F32, "r_lt")
    r_junk = kb.sb([128, NE], F32, "r_junk")
    destI = kb.sb([128, NT, 8], I32, "destI")
    w8 = kb.sb([128, NT, 8], F32, "w8")
    zrow = pT[0]
    for k in range(8):
        kb.dma(pool, lambda k=k: pool.e.dma_start(out=w_router[:, k, :], in_=w_router_d[k * 128:(k + 1) * 128, :]),
               writes=[w_router])
        kb.dma(pool, lambda k=k: pool.e.dma_start(out=ws_gate[:, k, :], in_=ws_gate_d[k * 128:(k + 1) * 128, :]),
               writes=[ws_gate])
        kb.dma(pool, lambda k=k: pool.e.dma_start(out=ws_up[:, k, :], in_=ws_up_d[k * 128:(k + 1) * 128, :]),
               writes=[ws_up])
    for c in range(2):
        kb.dma(pool, lambda c=c: pool.e.dma_start(out=ws_down[:, c, :], in_=ws_down_d[c * 128:(c + 1) * 128, :]),
               writes=[ws_down])
    kb.dma(sp, lambda: sp.e.dma_start(out=rbias[:], in_=rbias_d.partition_broadcast(128)), writes=[rbias])
    kb.dma(sp, lambda: sp.e.dma_start(out=ebase[:], in_=ebase_d.partition_broadcast(128)), writes=[ebase])
    kb.dma(sp, lambda: sp.e.dma_start(out=dummycol[:], in_=dummycol_d), writes=[dummycol])
    kb.dma(pool, lambda: pool.e.dma_start(out=ustrict[:], in_=ustrict_d), writes=[ustrict])
    kb.dma(sp, lambda: sp.e.dma_start(out=tokid[:], in_=tokid_d), writes=[tokid])
    kb.dma(sp, lambda: sp.e.dma_start(out=ln2g[:], in_=ln2g_d.partition_broadcast(128)), writes=[ln2g])
    kb.dma(sp, lambda: sp.e.dma_start(out=ln2b[:], in_=ln2b_d.partition_broadcast(128)), writes=[ln2b])
    fillv = junk2.t[:].bitcast(I32)
    kb.op(dve, lambda: dve.e.memset(fillv, NTOK), writes=[junk2])
    kb.op(dve, lambda: dve.e.memset(zrow[:], 0.0), writes=[zrow])
    kb.op(dve, lambda: dve.e.memset(r_Msum[:], 0.0), writes=[r_Msum])
    st_flat = slot_tok_d.rearrange("(p r) c -> p (r c)", p=128)
    for a in range(8):
        kb.dma(sp, lambda a=a: sp.e.dma_start(out=st_flat[:, a * 1024:(a + 1) * 1024], in_=fillv),
               reads=[junk2], writes=[slot_tok_b])
    kb.dma(sp, lambda: sp.e.dma_start(out=st_flat[:, 8192:8208], in_=fillv[:, 0:16]), reads=[junk2], writes=[slot_tok_b])
    kb.dma(sp, lambda: sp.e.dma_start(out=x1tab_d[NTOK:NTOK + 128, :], in_=zrow[:]), reads=[zrow], writes=[x1tab_b])
    kb.dma(sp, lambda: sp.e.dma_start(out=yslots_d[NSLOT:NSLOT + 128, :], in_=zrow[:]), reads=[zrow], writes=[yslots_b])

    def phase_moe1(t):
        kb.op(act, lambda: act.e.activation(out=x1b[:], in_=x1[:], func=AF.Copy), reads=[x1], writes=[x1b])
        kb.dma(sp, lambda: sp.e.dma_start(out=x1tab_d[t * 128:(t + 1) * 128, :], in_=x1b[:]),
               reads=[x1b], writes=[x1tab_b])
        p6 = PS[6]
        kb.group(pe, [lambda k=k: pe.e.transpose(out=psb(p6, k * 128, (k + 1) * 128),
                                                in_=x1b[:, k * 128:(k + 1) * 128], identity=identb[:])
                      for k in range(8)], reads=[x1b, identb], writes=[p6])
        kb.op(act, lambda: act.e.activation(out=x1T[:], in_=psb(p6, 0, 1024).rearrange("p (k n) -> p k n", k=8),
                                            func=AF.Copy), reads=[p6], writes=[x1T])
        p7 = PS[7]
        kb.group(pe, [lambda k=k: pe.e.matmul(p7[:, 0:NE], lhsT=x1T[:, k, :], rhs=w_router[:, k, :],
                                             start=(k == 0), stop=(k == 7)) for k in range(8)],
                 reads=[x1T, w_router], writes=[p7])
        p0 = PS[0]
        for m, wm in enumerate((ws_gate, ws_up)):
            for c in range(2):
                kb.group(pe, [lambda k=k, m=m, c=c, wm=wm: pe.e.matmul(
                    p0[:, (m * 2 + c) * 128:(m * 2 + c + 1) * 128], lhsT=wm[:, k, c * 128:(c + 1) * 128],
                    rhs=x1T[:, k, :], start=(k == 0), stop=(k == 7)) for k in range(8)],
                    reads=[wm, x1T], writes=[p0])
        kb.op(act, lambda: act.e.activation(out=sg[:, 0:256], in_=p0[:, 0:256], func=AF.Silu),
              reads=[p0], writes=[sg])
        kb.op(dve, lambda: dve.e.tensor_tensor(out=hTs[:].rearrange("p c n -> p (c n)"), in0=sg[:, 0:256],
                                               in1=p0[:, 256:512], op=ALU.mult), reads=[sg, p0], writes=[hTs])
        for half in range(2):
            kb.group(pe, [lambda c=c, half=half: pe.e.matmul(
                PS[2 + half][:, 0:512], lhsT=hTs[:, c, :], rhs=ws_down[:, c, half * 512:(half + 1) * 512],
                start=(c == 0), stop=(c == 1)) for c in range(2)], reads=[hTs, ws_down], writes=[PS[2 + half]])
            kb.op(dve, lambda half=half: dve.e.scalar_tensor_tensor(
                out=pre2[:, half * 512:(half + 1) * 512], in0=x1[:, half * 512:(half + 1) * 512],
                scalar=float(ALPHA), in1=PS[2 + half][:, 0:512], op0=ALU.mult, op1=ALU.add),
                reads=[x1, PS[2 + half]], writes=[pre2])
        kb.dma(sp, lambda: sp.e.dma_start(out=pre2_d[t * 128:(t + 1) * 128, :], in_=pre2[:]),
               reads=[pre2], writes=[pre2_b])
        kb.op(act, lambda: act.e.activation(out=r_s[:], in_=p7[:, 0:NE], func=AF.Sigmoid), reads=[p7], writes=[r_s])
        kb.op(dve, lambda: dve.e.tensor_tensor(out=r_sb[:], in0=r_s[:], in1=rbias[:], op=ALU.add),
              reads=[r_s, rbias], writes=[r_sb])
        for g in range(8):
            kb.op(dve, lambda g=g: dve.e.max(out=r_gtop[:, g, :], in_=r_sb[:, g * 32:(g + 1) * 32]),
                  reads=[r_sb], writes=[r_gtop])
        kb.op(dve, lambda: dve.e.tensor_tensor(out=r_sm[:, 0:8], in0=r_gtop[:, :, 0], in1=r_gtop[:, :, 1], op=ALU.add),
              reads=[r_gtop], writes=[r_sm])
        kb.op(dve, lambda: dve.e.max(out=r_sm[:, 8:16], in_=r_sm[:, 0:8]), reads=[r_sm], writes=[r_sm])
        kb.op(dve, lambda: dve.e.tensor_scalar(out=r_sm[:, 16:24], in0=r_sm[:, 0:8], scalar1=r_sm[:, 11:12],
                                               scalar2=None, op0=ALU.is_ge), reads=[r_sm], writes=[r_sm])
        kb.op(dve, lambda: dve.e.tensor_scalar(out=r_sm[:, 24:32], in0=r_sm[:, 16:24], scalar1=-1.0, scalar2=1e30,
                                               op0=ALU.add, op1=ALU.mult), reads=[r_sm], writes=[r_sm])
        v3 = lambda b: b[:, :].rearrange("p (g e) -> p g e", g=8)
        kb.op(dve, lambda: dve.e.tensor_tensor(out=v3(r_sbm), in0=v3(r_sb),
                                               in1=r_sm[:, 16:24].unsqueeze(2).to_broadcast([128, 8, 32]),
                                               op=ALU.mult), reads=[r_sb, r_sm], writes=[r_sbm])
        kb.op(dve, lambda: dve.e.tensor_tensor(out=v3(r_sbm), in0=v3(r_sbm),
                                               in1=r_sm[:, 24:32].unsqueeze(2).to_broadcast([128, 8, 32]),
                                               op=ALU.add), reads=[r_sbm, r_sm], writes=[r_sbm])
        kb.op(dve, lambda: dve.e.max(out=r_sm[:, 32:40], in_=r_sbm[:]), reads=[r_sbm], writes=[r_sm])
        kb.op(dve, lambda: dve.e.tensor_scalar(out=r_M[:], in0=r_sbm[:], scalar1=r_sm[:, 39:40], scalar2=None,
                                               op0=ALU.is_ge), reads=[r_sbm, r_sm], writes=[r_M])
        kb.op(dve, lambda: dve.e.tensor_tensor(out=r_W[:], in0=r_s[:], in1=r_M[:], op=ALU.mult),
              reads=[r_s, r_M], writes=[r_W])
        kb.op(dve, lambda: dve.e.tensor_reduce(out=r_sm[:, 48:49], in_=r_W[:], axis=AX.X, op=ALU.add),
              reads=[r_W], writes=[r_sm])
        kb.op(dve, lambda: dve.e.reciprocal(out=r_sm[:, 49:50], in_=r_sm[:, 48:49]), reads=[r_sm], writes=[r_sm])
        kb.op(dve, lambda: dve.e.tensor_scalar(out=r_W[:], in0=r_W[:], scalar1=r_sm[:, 49:50],
                                               scalar2=float(ROUTED_SCALE), op0=ALU.mult, op1=ALU.mult),
              reads=[r_W, r_sm], writes=[r_W])
        p1 = PS[1]
        kb.group(pe, [lambda: pe.e.matmul(p1[:, 0:NE], lhsT=ustrict[:], rhs=r_M[:], start=True, stop=False),
                      lambda: pe.e.matmul(p1[:, 0:NE], lhsT=ones_b[:], rhs=r_Msum[:], start=False, stop=True)],
                 reads=[ustrict, r_M, ones_b, r_Msum], writes=[p1])
        kb.op(dve, lambda: dve.e.tensor_tensor(out=r_Msum[:], in0=r_Msum[:], in1=r_M[:], op=ALU.add),
              reads=[r_Msum, r_M], writes=[r_Msum])
        kb.op(dve, lambda: dve.e.tensor_tensor(out=r_v[:], in0=p1[:, 0:NE], in1=ebase[:], op=ALU.add),
              reads=[p1, ebase], writes=[r_v])
        kb.op(dve, lambda: dve.e.tensor_scalar(out=r_lt[:], in0=p1[:, 0:NE], scalar1=float(CAP), scalar2=None,
                                               op0=ALU.is_lt), reads=[p1], writes=[r_lt])
        kb.op(dve, lambda: dve.e.tensor_tensor(out=r_lt[:], in0=r_lt[:], in1=r_M[:], op=ALU.mult),
              reads=[r_lt, r_M], writes=[r_lt])
        kb.op(dve, lambda: dve.e.tensor_tensor(out=r_v[:], in0=r_v[:], in1=r_lt[:], op=ALU.mult),
              reads=[r_v, r_lt], writes=[r_v])
        kb.op(dve, lambda: dve.e.max(out=r_sm[:, 40:48], in_=r_v[:]), reads=[r_v], writes=[r_sm])
        for k in range(8):
            kb.op(dve, lambda k=k: dve.e.scalar_tensor_tensor(
                out=r_junk[:], in0=r_v[:], scalar=r_sm[:, 40 + k:41 + k], in1=r_W[:], op0=ALU.is_equal,
                op1=ALU.mult, accum_out=w8[:, t, k:k + 1]), reads=[r_v, r_sm, r_W], writes=[r_junk, w8])
        kb.op(dve, lambda: dve.e.tensor_scalar(out=r_gtop[:, 0, :], in0=r_sm[:, 40:48], scalar1=0.0, scalar2=None,
                                               op0=ALU.is_equal), reads=[r_sm], writes=[r_gtop])
        kb.op(dve, lambda: dve.e.scalar_tensor_tensor(out=r_gtop[:, 1, :], in0=r_gtop[:, 0, :], scalar=dummycol[:, 0:1],
                                                      in1=r_sm[:, 40:48], op0=ALU.mult, op1=ALU.add),
              reads=[r_gtop, dummycol, r_sm], writes=[r_gtop])
        kb.op(dve, lambda: dve.e.tensor_scalar(out=destI[:, t, :], in0=r_gtop[:, 1, :], scalar1=-1.0, scalar2=None,
                                               op0=ALU.add), reads=[r_gtop], writes=[destI])
        for k in range(8):
            kb.dma(pool, lambda k=k: pool.e.indirect_dma_start(
                out=slot_tok_d[:, :], out_offset=bass.IndirectOffsetOnAxis(ap=destI[:, t, k:k + 1], axis=0),
                in_=tokid[:, t, :], in_offset=None), reads=[destI, tokid], writes=[slot_tok_b])

    ids_sb = [kb.sb([128, 2, 16], I32, f"ids{i}") for i in range(2)]
    hT = kb.sb([128, 2, 256], BF16, "hT")

    def alloc_moe_aliases():
        nonlocal xg, xgT, wg, wu, wd, yb, acc, yg, outt
        xg = [kb.alias(xin[i], xin[i].t[:].bitcast(BF16).rearrange("p (j d) -> p j d", j=2)) for i in range(2)]
        wflat = w_in.t[:].rearrange("p k c -> p (k c)")
        wv = lambda n: wflat[:, n * 2048:(n + 1) * 2048]
        wg = [kb.alias(w_in, wv(3 * i + 0).rearrange("p (k f) -> p k f", k=8)) for i in range(2)]
        wu = [kb.alias(w_in, wv(3 * i + 1).rearrange("p (k f) -> p k f", k=8)) for i in range(2)]
        wd = [kb.alias(w_in, wv(3 * i + 2).rearrange("p (c d) -> p c d", c=2)) for i in range(2)]
        yb = [kb.alias(ckvT[i], ckvT[i].t[:].rearrange("p (j d) -> p j d", j=2)) for i in range(2)]
        xgT = kb.alias(ckvE[0], ckvE[0].t[:].rearrange("p a b -> p (a b)").rearrange("p (k n) -> p k n", k=8))
        acc = kb.alias(kpeT[0], kpeT[0].t[:].bitcast(F32))
        outt = kb.alias(kpeT[1], kpeT[1].t[:].bitcast(F32))
        e1 = ckvE[1].t[:].rearrange("p a b -> p (a b)")
        yg = [kb.alias(ckvE[1], e1[:, 0:1024]), kb.alias(ckvE[1], e1[:, 1024:2048]),
              kb.alias(o_latT, o_latT.t[:].rearrange("p a b -> p (a b)")),
              kb.alias(q_latT, q_latT.t[:].rearrange("p a b -> p (a b)"))]

    xg = xgT = wg = wu = wd = yb = acc = yg = outt = None

    def moe_load(e):
        i = e % 2
        kb.dma(pool, lambda: pool.e.dma_start(out=wg[i][:], in_=w_gate_d[e].rearrange("p (k f) -> p k f", k=8)),
               writes=[wg[i]])
        kb.dma(pool, lambda: pool.e.dma_start(out=wu[i][:], in_=w_up_d[e].rearrange("p (k f) -> p k f", k=8)),
               writes=[wu[i]])
        kb.dma(pool, lambda: pool.e.dma_start(out=wd[i][:], in_=w_down_d[e].rearrange("p (c d) -> p c d", c=2)),
               writes=[wd[i]])
        kb.dma(sp, lambda: sp.e.dma_start(
            out=ids_sb[i][:], in_=slot_tok_d[e * CAP:(e + 1) * CAP, :].rearrange("(p j) c -> p j c", j=2)),
            reads=[slot_tok_b], writes=[ids_sb[i]])
        for j in range(2):
            kb.dma(pool, lambda j=j: pool.e.indirect_dma_start(
                out=xg[i][:, j, :], out_offset=None, in_=x1tab_d[:, :],
                in_offset=bass.IndirectOffsetOnAxis(ap=ids_sb[i][:, j, 0:1], axis=0)),
                reads=[ids_sb[i], x1tab_b], writes=[xg[i]])

    def moe_T(e):
        i = e % 2
        for j in range(2):
            pj = PS[j]
            kb.group(pe, [lambda k=k, j=j, pj=pj: pe.e.transpose(
                out=psb(pj, k * 128, (k + 1) * 128), in_=xg[i][:, j, k * 128:(k + 1) * 128], identity=identb[:])
                for k in range(8)], reads=[xg[i], identb], writes=[pj])
            if j == 0:
                kb.op(act, lambda j=j, pj=pj: act.e.activation(
                    out=xgT[:, :, j * 128:(j + 1) * 128], in_=psb(pj, 0, 1024).rearrange("p (k n) -> p k n", k=8),
                    func=AF.Copy), reads=[pj], writes=[xgT])
            else:
                kb.op(dve, lambda j=j, pj=pj: dve.e.tensor_copy(
                    out=xgT[:, :, j * 128:(j + 1) * 128], in_=psb(pj, 0, 1024).rearrange("p (k n) -> p k n", k=8)),
                    reads=[pj], writes=[xgT])

    def moe_GU(e):
        i = e % 2
        for m, wm in enumerate((wg[i], wu[i])):
            for c in range(2):
                kb.group(pe, [lambda k=k, m=m, c=c, wm=wm: pe.e.matmul(
                    PS[2 + m][:, c * 256:(c + 1) * 256], lhsT=wm[:, k, c * 128:(c + 1) * 128], rhs=xgT[:, k, :],
                    start=(k == 0), stop=(k == 7)) for k in range(8)], reads=[wm, xgT], writes=[PS[2 + m]])
        kb.op(act, lambda: act.e.activation(out=sg[:], in_=PS[2][:, :], func=AF.Silu), reads=[PS[2]], writes=[sg])
        kb.op(dve, lambda: dve.e.tensor_tensor(out=hT[:].rearrange("p c n -> p (c n)"), in0=sg[:], in1=PS[3][:, :],
                                               op=ALU.mult), reads=[sg, PS[3]], writes=[hT])

    def moe_D(e):
        i = e % 2
        for j in range(2):
            for half in range(2):
                pb = PS[4 + 2 * j + half]
                kb.group(pe, [lambda c=c, j=j, half=half, pb=pb: pe.e.matmul(
                    pb[:, 0:512], lhsT=hT[:, c, j * 128:(j + 1) * 128], rhs=wd[i][:, c, half * 512:(half + 1) * 512],
                    start=(c == 0), stop=(c == 1)) for c in range(2)], reads=[hT, wd[i]], writes=[pb])
                if half == 0:
                    kb.op(act, lambda j=j, half=half, pb=pb: act.e.activation(
                        out=yb[i][:, j, half * 512:(half + 1) * 512], in_=pb[:, 0:512], func=AF.Copy),
                        reads=[pb], writes=[yb[i]])
                else:
                    kb.op(dve, lambda j=j, half=half, pb=pb: dve.e.tensor_copy(
                        out=yb[i][:, j, half * 512:(half + 1) * 512], in_=pb[:, 0:512]),
                        reads=[pb], writes=[yb[i]])
        kb.dma(sp, lambda: sp.e.dma_start(
            out=yslots_d[e * CAP:(e + 1) * CAP, :].rearrange("(p j) d -> p j d", j=2), in_=yb[i][:]),
            reads=[yb[i]], writes=[yslots_b])

    def phase_moe3(t):
        kb.dma(sp, lambda: sp.e.dma_start(out=acc[:], in_=pre2_d[t * 128:(t + 1) * 128, :]),
               reads=[pre2_b], writes=[acc])
        for k in range(8):
            g = yg[k % 4]
            kb.dma(pool, lambda k=k, g=g: pool.e.indirect_dma_start(
                out=g[:], out_offset=None, in_=yslots_d[:, :],
                in_offset=bass.IndirectOffsetOnAxis(ap=destI[:, t, k:k + 1], axis=0)),
                reads=[destI, yslots_b], writes=[g])
            kb.op(dve, lambda k=k, g=g: dve.e.scalar_tensor_tensor(
                out=acc[:], in0=g[:], scalar=w8[:, t, k:k + 1], in1=acc[:], op0=ALU.mult, op1=ALU.add),
                reads=[g, w8, acc], writes=[acc])
        layer_norm(acc, outt, ln2g, ln2b)
        dst = y_s if t == NPT else y_p[t * 128:(t + 1) * 128, :]
        kb.dma(sp, lambda: sp.e.dma_start(out=dst, in_=outt[:]), reads=[outt])


    for t in (range(KNT) if KNT else range(NT)):
        phase_a(t)
        if STAGE >= 2 and t < NPT:
            if SUB >= 2:
                phase_b(t)
            if SUB >= 3:
                phase_c(t)
            if STAGE == 2 and SUB >= 3:
                kb.dma(sp, lambda t=t: sp.e.dma_start(out=y_p[t * 128:(t + 1) * 128, :], in_=x1[:]), reads=[x1])
            if STAGE >= 5:
                phase_moe1(t)
        if STAGE >= 3 and t == NPT:
            phase_bs()
            phase_c(t)
            if STAGE == 3:
                kb.dma(sp, lambda: sp.e.dma_start(out=y_s, in_=x1[:]), reads=[x1])
            if STAGE >= 5:
                phase_moe1(t)
    if STAGE >= 5:
        tiles = list(range(KNT) if KNT else range(NT))
        nexp = int(os.environ.get('KNE', str(NE)))
        alloc_moe_aliases()
        moe_load(0)
        for e in range(nexp):
            if e + 1 < nexp:
                moe_load(e + 1)
            moe_T(e)
            moe_GU(e)
            moe_D(e)
        for t in tiles:
            phase_moe3(t)

    if STAGE < 9:
        zt = kb.sb([128, D], F32, "zt")
        kb.op(dve, lambda: dve.e.memset(zt[:], 0.0), writes=[zt])
        if STAGE < 2 or KNT:
            for t in range(KNT if STAGE >= 2 else 0, NPT):
                kb.dma(sp, lambda t=t: sp.e.dma_start(out=y_p[t * 128:(t + 1) * 128, :], in_=zt[:]), reads=[zt])
        if STAGE < 3 or KNT:
            kb.dma(sp, lambda: sp.e.dma_start(out=y_s, in_=zt[:]), reads=[zt])

    kb.finish()
    nc._in_names = in_names
    return nc


_PROGRAM = None


def _get_program():
    global _PROGRAM
    if _PROGRAM is None:
        _PROGRAM = build_program()
    return _PROGRAM


def _rope_tables(pos):
    half = ROPE // 2
    inv = (np.float32(10000.0) ** (-np.arange(half, dtype=np.float32) / np.float32(half))).astype(np.float32)
    ang = pos.astype(np.float32)[:, None] * inv[None, :]
    return np.cos(ang).astype(np.float32), np.sin(ang).astype(np.float32)


def kernel(x_prompt, x_sample, cache_kv_latent, cache_k_rope, state_conv, page_table,
           w_in, conv_w, q_norm, w_uq, kv_norm, w_uk, w_uv, g_conv, g_attn, w_o,
           ln1_g, ln1_b, w_router, router_bias, w_gate, w_up, w_down,
           ws_gate, ws_up, ws_down, ln2_g, ln2_b):
    f = lambda a: np.ascontiguousarray(np.asarray(a), dtype=np.float32)
    x_prompt = f(x_prompt); x_sample = f(x_sample)
    w_uq0 = f(w_uq)[0].reshape(QL, NH, NOPE + ROPE)
    w_uq_p = np.ascontiguousarray(np.concatenate(
        [w_uq0[:, :, :NOPE].reshape(QL, NH * NOPE), w_uq0[:, :, NOPE:].reshape(QL, NH * ROPE)], axis=1))
    w_uk0 = f(w_uk)[0]
    w_ukT = np.zeros((128, NH, KVL), np.float32)
    w_uvz = np.zeros((KVL, NH, 128), np.float32)
    w_uv0 = f(w_uv)[0]
    for h in range(NH):
        w_ukT[(h % 2) * NOPE:(h % 2 + 1) * NOPE, h, :] = w_uk0[:, h, :].T
        w_uvz[:, h, (h % 2) * VD:(h % 2 + 1) * VD] = w_uv0[:, h, :]
    rowmask = np.zeros((128, 4), np.float32)
    for i in range(4):
        rowmask[i * 32:(i + 1) * 32, i] = 1.0
    gmix = np.ascontiguousarray(np.concatenate([f(g_conv)[0], f(g_attn)[0]]).reshape(8, 128).T)
    qn = np.ascontiguousarray(f(q_norm)[0].reshape(2, 128).T)
    cw = np.ascontiguousarray(f(conv_w)[0].reshape(3, 4, 128).transpose(2, 1, 0))
    cos_p, sin_p = _rope_tables(np.arange(SEQ))
    cos_p = np.ascontiguousarray(cos_p.reshape(NQB, 128, 16).transpose(1, 0, 2))
    sin_p = np.ascontiguousarray(sin_p.reshape(NQB, 128, 16).transpose(1, 0, 2))
    cos_s, sin_s = _rope_tables(PAST + (np.arange(128) % DT))
    ident = np.eye(128, dtype=np.float32)
    cmask = (np.arange(128)[:, None] <= np.arange(128)[None, :]).astype(np.float32)
    ustrict = (np.arange(128)[:, None] < np.arange(128)[None, :]).astype(np.float32)
    ebase = (np.arange(NE, dtype=np.float32) * CAP + 1.0).reshape(1, NE)
    dummycol = (NSLOT + 1 + np.arange(128, dtype=np.float32)).reshape(128, 1)
    tokid = np.ascontiguousarray(np.broadcast_to(
        (np.arange(NT, dtype=np.int32)[None, :, None] * 128 + np.arange(128, dtype=np.int32)[:, None, None]),
        (128, NT, 16)))
    common = {
        "ustrict": ustrict, "ebase": ebase, "dummycol": dummycol, "tokid": tokid,
        "w_in": f(w_in)[0], "w_uq": w_uq_p, "w_ukT": w_ukT, "w_uv": w_uvz, "rowmask": rowmask,
        "w_o": f(w_o)[0], "gmix": gmix, "qnorm": qn, "kvnorm": f(kv_norm)[0].reshape(1, KVL), "convw": cw,
        "ln1g": f(ln1_g)[0].reshape(1, D), "ln1b": f(ln1_b)[0].reshape(1, D),
        "ln2g": f(ln2_g)[0].reshape(1, D), "ln2b": f(ln2_b)[0].reshape(1, D),
        "w_router": f(w_router)[0], "rbias": f(router_bias)[0].reshape(1, NE),
        "ws_gate": f(ws_gate)[0], "ws_up": f(ws_up)[0], "ws_down": f(ws_down)[0],
        "w_gate": np.ascontiguousarray(f(w_gate)[0].reshape(NE, 8, 128, 256).transpose(0, 2, 1, 3)).reshape(NE, 128, 2048),
        "w_up": np.ascontiguousarray(f(w_up)[0].reshape(NE, 8, 128, 256).transpose(0, 2, 1, 3)).reshape(NE, 128, 2048),
        "w_down": np.ascontiguousarray(f(w_down)[0].reshape(NE, 2, 128, D).transpose(0, 2, 1, 3)).reshape(NE, 128, 2048),
        "cache_all": np.ascontiguousarray(np.concatenate(
            [f(cache_kv_latent)[0].reshape(NPOOL * 128, KVL), f(cache_k_rope)[0].reshape(NPOOL * 128, ROPE)], axis=1)),
        "cosp": cos_p, "sinp": sin_p, "coss": cos_s, "sins": sin_s, "identf": ident, "cmask": cmask,
    }
    smask = np.zeros((128, DB, DT), np.float32)
    for b in range(DB):
        for tq in range(DT):
            smask[b * DT:b * DT + tq + 1, b, tq] = 1.0
    common["smask"] = smask
    common["pcol"] = np.arange(128, dtype=np.float32).reshape(128, 1)
    pt = np.ascontiguousarray(np.asarray(page_table), dtype=np.int32)
    sc = f(state_conv)[0]
    in_maps = []
    for c in range(NCORES):
        m = dict(common)
        m["xp"] = x_prompt[NSEQ * c:NSEQ * (c + 1)].reshape(NPT * 128, D)
        m["xs"] = x_sample[DB * c:DB * (c + 1)].reshape(128, D)
        m["ptab"] = pt[DB * c:DB * (c + 1)].reshape(1, DB * NPAGE)
        m["sconv"] = sc[DB * c:DB * (c + 1)]
        in_maps.append(m)
    nc = _get_program()
    in_maps = [{k: v for k, v in m.items() if k in nc._in_names} for m in in_maps]
    kne = int(os.environ.get('KNE', str(NE)))
    if kne != NE:
        for m in in_maps:
            for k in ("w_gate", "w_up", "w_down"):
                m[k] = m[k][:kne]
    res = run_bass_kernel_spmd(nc, in_maps, core_ids=list(range(NCORES)))
    R = res.results
    cat = lambda k: np.concatenate([np.asarray(R[c][k]) for c in range(NCORES)], axis=0)
    y_prompt = cat("y_p").reshape(16, SEQ, D)
    y_sample = cat("y_s").reshape(128, DT, D)
    p_kv = cat("kv_p").reshape(1, 16, SEQ, KVL)
    p_kr = cat("kr_p").reshape(1, 16, SEQ, ROPE)
    p_cs = cat("cs_p").reshape(1, 16, 2, CONV_CH)
    s_kv = cat("kv_s").reshape(1, 128, DT, KVL)
    s_kr = cat("kr_s").reshape(1, 128, DT, ROPE)
    s_cs = cat("cs_s").reshape(1, 128, 2, CONV_CH)
    return (y_prompt, y_sample, p_kv, p_kr, p_cs, s_kv, s_kr, s_cs)
```
